# Optimizing a Trainium2 kernel written in Bass

```python
import jax, jax.numpy as jnp
from jax import lax
import numpy as np

D_MODEL = 1024
BATCH = 2
SEQ = 8192
DEPTH = 2

HEAD_DIM = 64
ROPE_THETA = 10000.0

RET_HEADS = 4
RET_DK = 64
RET_DV = 64
RET_CHUNK = 128

SWA_Q_HEADS = 6
SWA_KV_HEADS = 2
SWA_WINDOW = 128
SWA_BLOCK = 128

MLA_HEADS = 6
MLA_Q_RANK = 384
MLA_KV_RANK = 256
MLA_NOPE = 64
MLA_ROPE = 32
MLA_V = 64
MLA_BLOCK = 128

RET_W = RET_HEADS * RET_DV
SWA_W = SWA_Q_HEADS * HEAD_DIM
MLA_W = MLA_HEADS * MLA_V
D_MIX = RET_W + SWA_W + MLA_W

IN_SIZES = (
    RET_HEADS * RET_DK,
    RET_HEADS * RET_DK,
    RET_W,
    RET_W,
    SWA_Q_HEADS * HEAD_DIM,
    SWA_KV_HEADS * HEAD_DIM,
    SWA_KV_HEADS * HEAD_DIM,
    MLA_Q_RANK,
    MLA_KV_RANK,
    MLA_ROPE,
)
IN_COLS = sum(IN_SIZES)

N_EXPERTS = 16
N_GROUPS = 4
EXPERTS_PER_GROUP = N_EXPERTS // N_GROUPS
TOP_K = 2
D_EXPERT = 256

DEEPNORM_ALPHA = (2 * DEPTH) ** 0.25
DEEPNORM_BETA = (8 * DEPTH) ** -0.25
LN_EPS = 1e-5
RMS_EPS = 1e-6
NEG_INF = -1e30

kernel_name = "hymba_style_ret_swa_mla_grouped_moe_deepnorm"


def _split_points():
    return [int(s) for s in np.cumsum(np.array(IN_SIZES))[:-1]]


def layer_norm(x, g, b):
    xf = x.astype(jnp.float32)
    mu = jnp.mean(xf, -1, keepdims=True)
    var = jnp.mean(jnp.square(xf - mu), -1, keepdims=True)
    return ((xf - mu) * lax.rsqrt(var + LN_EPS) * g + b).astype(x.dtype)


def rms_norm(x, g):
    xf = x.astype(jnp.float32)
    return (xf * lax.rsqrt(jnp.mean(xf * xf, -1, keepdims=True) + RMS_EPS) * g).astype(x.dtype)


def rope(x, pos):
    d = x.shape[-1]
    inv = ROPE_THETA ** (-jnp.arange(0, d, 2, dtype=jnp.float32) / d)
    ang = pos.astype(jnp.float32)[:, None] * inv[None, :]
    cos = jnp.cos(ang)[:, None, :]
    sin = jnp.sin(ang)[:, None, :]
    x1, x2 = x[..., : d // 2], x[..., d // 2:]
    return jnp.concatenate([x1 * cos - x2 * sin, x1 * sin + x2 * cos], -1).astype(x.dtype)


def retention(q, k, v, g, gn_w):
    B, S, H, dk = q.shape
    dv = RET_DV
    C = RET_CHUNK
    NC = S // C
    gamma = 1.0 - 2.0 ** (-5.0 - jnp.arange(H, dtype=jnp.float32))
    log_g = jnp.log(gamma)
    idx = jnp.arange(C, dtype=jnp.float32)
    rel = idx[:, None] - idx[None, :]
    decay_in = jnp.where(rel[None] >= 0,
                         jnp.exp(jnp.maximum(rel, 0.0)[None] * log_g[:, None, None]), 0.0)
    xi = jnp.exp((idx[None, :] + 1.0) * log_g[:, None]).T
    zeta = jnp.exp((C - 1.0 - idx[None, :]) * log_g[:, None]).T
    chunk_decay = jnp.exp(C * log_g)

    qc = q.reshape(B, NC, C, H, dk).astype(jnp.float32)
    kc = (k * (dk ** -0.5)).reshape(B, NC, C, H, dk).astype(jnp.float32)
    vc = v.reshape(B, NC, C, H, dv).astype(jnp.float32)

    scores = jnp.einsum('bnihd,bnjhd->bnhij', qc, kc) * decay_in[None, None]
    inner = jnp.einsum('bnhij,bnjhe->bnihe', scores, vc)

    upd = jnp.einsum('bnjhd,bnjhe->bnhde', kc * zeta[None, None, :, :, None], vc)

    def step(state, u):
        return chunk_decay[None, :, None, None] * state + u, state

    init = jnp.zeros((B, H, dk, dv), jnp.float32)
    _, prev_states = lax.scan(step, init, jnp.moveaxis(upd, 1, 0))
    prev_states = jnp.moveaxis(prev_states, 0, 1)
    cross = jnp.einsum('bnihd,bnhde->bnihe', qc * xi[None, None, :, :, None], prev_states)

    o = (inner + cross).reshape(B, S, H, dv)
    mu = jnp.mean(o, -1, keepdims=True)
    var = jnp.mean(jnp.square(o - mu), -1, keepdims=True)
    o = (o - mu) * lax.rsqrt(var + LN_EPS) * gn_w.astype(jnp.float32).reshape(H, dv)
    o = o.reshape(B, S, H * dv) * jax.nn.silu(g.astype(jnp.float32))
    return o.astype(q.dtype)


def sliding_window_sink_attention(q, k, v, sinks):
    B, S, Hq, d = q.shape
    Hkv = k.shape[2]
    G = Hq // Hkv
    L = SWA_BLOCK
    NB = S // L
    qb = q.reshape(B, NB, L, Hkv, G, d)
    kb = k.reshape(B, NB, L, Hkv, d)
    vb = v.reshape(B, NB, L, Hkv, d)
    pad = ((0, 0), (1, 0), (0, 0), (0, 0), (0, 0))
    kwin = jnp.concatenate([jnp.pad(kb, pad)[:, :NB], kb], axis=2)
    vwin = jnp.concatenate([jnp.pad(vb, pad)[:, :NB], vb], axis=2)

    s = jnp.einsum('bnihgd,bnjhd->bnhgij', qb, kwin).astype(jnp.float32) * (d ** -0.5)
    i = jnp.arange(L)[:, None]
    j = jnp.arange(2 * L)[None, :]
    band = (j <= i + L) & (j > i + L - SWA_WINDOW)
    blk = jnp.arange(NB)[:, None]
    valid = (blk > 0) | (jnp.arange(2 * L)[None, :] >= L)
    mask = band[None] & valid[:, None, :]
    s = jnp.where(mask[None, :, None, None], s, NEG_INF)

    sink = sinks.astype(jnp.float32).reshape(1, 1, Hkv, G, 1, 1)
    m = jnp.maximum(jnp.max(s, -1, keepdims=True), sink)
    p = jnp.exp(s - m)
    denom = jnp.sum(p, -1, keepdims=True) + jnp.exp(sink - m)
    o = jnp.einsum('bnhgij,bnjhd->bnihgd', (p / denom).astype(v.dtype), vwin)
    return o.reshape(B, S, Hq * d)


def latent_attention(c_q, c_kv, k_rope, q_norm_w, kv_norm_w, w_uq, w_ukv, pos):
    B, S, _ = c_q.shape
    H = MLA_HEADS
    dq = MLA_NOPE + MLA_ROPE
    q = (rms_norm(c_q, q_norm_w) @ w_uq).reshape(B, S, H, dq)
    q = jnp.concatenate([q[..., :MLA_NOPE], rope(q[..., MLA_NOPE:], pos)], -1)
    kv = (rms_norm(c_kv, kv_norm_w) @ w_ukv).reshape(B, S, H, MLA_NOPE + MLA_V)
    k_nope, v = kv[..., :MLA_NOPE], kv[..., MLA_NOPE:]
    k_pe = rope(k_rope[:, :, None, :], pos)
    k = jnp.concatenate([k_nope, jnp.broadcast_to(k_pe, (B, S, H, MLA_ROPE))], -1)
    scale = dq ** -0.5
    L = MLA_BLOCK
    NB = S // L
    qb = jnp.moveaxis(q.reshape(B, NB, L, H, dq), 1, 0)
    kpos = jnp.arange(S)

    def block(args):
        q_blk, n = args
        s = jnp.einsum('bihd,bjhd->bhij', q_blk, k).astype(jnp.float32) * scale
        qpos = n * L + jnp.arange(L)
        s = jnp.where(kpos[None, :] <= qpos[:, None], s, NEG_INF)
        p = jax.nn.softmax(s, axis=-1)
        return jnp.einsum('bhij,bjhe->bihe', p.astype(v.dtype), v)

    o = lax.map(block, (qb, jnp.arange(NB)))
    return jnp.moveaxis(o, 0, 1).reshape(B, S, H * MLA_V)


def grouped_moe(x, router_w, router_bias, w_gate_up, w_down):
    B, S, D = x.shape
    t = x.reshape(-1, D)
    T = t.shape[0]
    scores = jax.nn.sigmoid((t @ router_w).astype(jnp.float32))
    biased = scores + router_bias.astype(jnp.float32)
    grp = biased.reshape(T, N_GROUPS, EXPERTS_PER_GROUP)
    grp_score = jnp.sum(lax.top_k(grp, TOP_K)[0], -1)
    sel_group = jnp.argmax(grp_score, -1)
    in_group = jnp.repeat(jax.nn.one_hot(sel_group, N_GROUPS) > 0, EXPERTS_PER_GROUP, axis=-1)
    _, top_idx = lax.top_k(jnp.where(in_group, biased, -jnp.inf), TOP_K)
    sel = jnp.take_along_axis(scores, top_idx, -1)
    weights = sel / jnp.sum(sel, -1, keepdims=True)
    gates = jnp.sum(jax.nn.one_hot(top_idx, N_EXPERTS, dtype=jnp.float32) * weights[..., None], 1)
    h = jnp.einsum('td,edf->tef', t, w_gate_up)
    a = jax.nn.silu(h[..., :D_EXPERT]) * h[..., D_EXPERT:] * gates[..., None].astype(h.dtype)
    y = jnp.einsum('tef,efd->td', a, w_down)
    return y.reshape(B, S, D)


def setup_inputs(seed: int = 0) -> dict:
    key = jax.random.key(seed)
    ks = jax.random.split(key, 20)
    f32 = jnp.float32
    nrm = lambda k, shape, s: jax.random.normal(k, shape, f32) * s
    return {
        "x": nrm(ks[0], (BATCH, SEQ, D_MODEL), 1.0),
        "w_in": nrm(ks[1], (DEPTH, D_MODEL, IN_COLS), D_MODEL ** -0.5),
        "ret_gn_w": 1.0 + nrm(ks[2], (DEPTH, RET_W), 0.02),
        "swa_sinks": nrm(ks[3], (DEPTH, SWA_Q_HEADS), 0.5),
        "mla_q_norm_w": 1.0 + nrm(ks[4], (DEPTH, MLA_Q_RANK), 0.02),
        "mla_kv_norm_w": 1.0 + nrm(ks[5], (DEPTH, MLA_KV_RANK), 0.02),
        "mla_w_uq": nrm(ks[6], (DEPTH, MLA_Q_RANK, MLA_HEADS * (MLA_NOPE + MLA_ROPE)), MLA_Q_RANK ** -0.5),
        "mla_w_ukv": nrm(ks[7], (DEPTH, MLA_KV_RANK, MLA_HEADS * (MLA_NOPE + MLA_V)), MLA_KV_RANK ** -0.5),
        "w_out": nrm(ks[8], (DEPTH, D_MIX, D_MODEL), DEEPNORM_BETA * D_MIX ** -0.5),
        "ln1_g": 1.0 + nrm(ks[9], (DEPTH, D_MODEL), 0.02),
        "ln1_b": nrm(ks[10], (DEPTH, D_MODEL), 0.02),
        "router_w": nrm(ks[11], (D_MODEL, N_EXPERTS), D_MODEL ** -0.5),
        "router_bias": nrm(ks[12], (N_EXPERTS,), 0.01),
        "exp_w_gate_up": nrm(ks[13], (DEPTH, N_EXPERTS, D_MODEL, 2 * D_EXPERT), D_MODEL ** -0.5),
        "exp_w_down": nrm(ks[14], (DEPTH, N_EXPERTS, D_EXPERT, D_MODEL), DEEPNORM_BETA * D_EXPERT ** -0.5),
        "ln2_g": 1.0 + nrm(ks[15], (DEPTH, D_MODEL), 0.02),
        "ln2_b": nrm(ks[16], (DEPTH, D_MODEL), 0.02),
    }


def reference(x, w_in, ret_gn_w, swa_sinks, mla_q_norm_w, mla_kv_norm_w, mla_w_uq, mla_w_ukv,
              w_out, ln1_g, ln1_b, router_w, router_bias, exp_w_gate_up, exp_w_down, ln2_g, ln2_b):
    B, S, _ = x.shape
    pos = jnp.arange(S)
    splits = _split_points()
    for l in range(DEPTH):
        (r_q, r_k, r_v, r_g, s_q, s_k, s_v, c_q, c_kv, k_rope) = jnp.split(x @ w_in[l], splits, axis=-1)
        rq = rope(r_q.reshape(B, S, RET_HEADS, RET_DK), pos)
        rk = rope(r_k.reshape(B, S, RET_HEADS, RET_DK), pos)
        ret_o = retention(rq, rk, r_v, r_g, ret_gn_w[l])
        swa_o = sliding_window_sink_attention(
            s_q.reshape(B, S, SWA_Q_HEADS, HEAD_DIM),
            s_k.reshape(B, S, SWA_KV_HEADS, HEAD_DIM),
            s_v.reshape(B, S, SWA_KV_HEADS, HEAD_DIM),
            swa_sinks[l])
        mla_o = latent_attention(c_q, c_kv, k_rope, mla_q_norm_w[l], mla_kv_norm_w[l],
                                 mla_w_uq[l], mla_w_ukv[l], pos)
        mix = jnp.concatenate([ret_o, swa_o, mla_o], axis=-1) @ w_out[l]
        x = layer_norm(DEEPNORM_ALPHA * x + mix, ln1_g[l], ln1_b[l])
        ffn = grouped_moe(x, router_w, router_bias, exp_w_gate_up[l], exp_w_down[l])
        x = layer_norm(DEEPNORM_ALPHA * x + ffn, ln2_g[l], ln2_b[l])
    return x
```

```python
import numpy as np
import ml_dtypes
import contextlib
import concourse.bass as bass
import concourse.mybir as mybir
from concourse.bass_utils import run_bass_kernel_spmd

F32 = mybir.dt.float32
BF16 = mybir.dt.bfloat16
U8 = mybir.dt.uint8
AF = mybir.ActivationFunctionType
ALU = mybir.AluOpType
AX = mybir.AxisListType

NT = 16
TOK = 2048
D = 1024
INC = 2336
ALPHA = 4.0 ** 0.25
LN_EPS = 1e-5
RMS_EPS = 1e-6
ENGS = ['pe', 'act', 'dve', 'pool', 'sp']
import os
KCUT = int(os.environ.get('KCUT', '0'))

EX_SK, EX_KN, EX_KP, EX_SV, EX_VM, EX_ROWS = 0, 128, 512, 544, 928, 1312
EXU_ROWS = 512


class Prog:
    def __init__(self, nc):
        self.nc = nc
        self.streams = {e: [] for e in ENGS}
        self.cnt = {e: 0 for e in ENGS}
        self.waited = {e: {} for e in ENGS}
        self.res = {}
        self.dcnt = {}

    def _deps(self, eng, reads, writes, dma_key=None):
        toks = []
        for r in reads:
            st = self.res.get(r)
            if st is not None and st[0] is not None:
                toks.append(st[0])
        for w in writes:
            st = self.res.get(w)
            if st is not None:
                if st[0] is not None and not (dma_key is not None and st[0][0] == 'd' and st[0][1] == dma_key):
                    toks.append(st[0])
                toks.extend(st[1])
        waits = []
        for t in toks:
            kind, p, c = t
            if kind == 'e' and p == eng and eng == 'pe':
                continue
            if kind == 'd':
                c = self.dcnt[p]
                t = (kind, p, c)
            k = (kind, p)
            if self.waited[eng].get(k, 0) >= c:
                continue
            self.waited[eng][k] = c
            waits.append(t)
        return waits

    def _upd(self, tok, reads, writes):
        for r in reads:
            if r in writes:
                continue
            st = self.res.setdefault(r, [None, []])
            st[1] = [x for x in st[1] if not (x[0] == tok[0] and x[1] == tok[1])] + [tok]
        for w in writes:
            self.res[w] = [tok, []]

    def op(self, eng, name, kw, reads=(), writes=()):
        fn = (lambda e, name=name, kw=kw: getattr(e, name)(**kw))
        waits = self._deps(eng, reads, writes)
        if eng == 'pe':
            src = kw.get('lhsT', kw.get('in_'))
            rg = src.base_partition() if callable(getattr(src, 'base_partition', None)) else 0
            if rg != getattr(self, '_last_rg', 0) and self.cnt['pe'] > 0:
                if self.waited['pe'].get(('e', 'pe'), 0) < self.cnt['pe']:
                    self.waited['pe'][('e', 'pe')] = self.cnt['pe']
                    waits.append(('e', 'pe', self.cnt['pe']))
            self._last_rg = rg
        self.cnt[eng] += 1
        tok = ('e', eng, self.cnt[eng])
        self.streams[eng].append((waits, fn, ('e', eng)))
        self._upd(tok, reads, writes)
        return tok

    def dma(self, q, out, in_, reads, writes, key):
        waits = self._deps(q, reads, writes, dma_key=key)
        self.dcnt[key] = self.dcnt.get(key, 0) + 16
        tok = ('d', key, self.dcnt[key])
        self.streams[q].append((waits, (lambda e, o=out, i=in_: e.dma_start(out=o, in_=i)), ('d', key)))
        self._upd(tok, reads, writes)
        return tok

    def custom(self, q, name, kw, reads, writes, key, incval):
        if os.environ.get('KNOCC'):
            return None
        fn = (lambda e, name=name, kw=kw: getattr(e, name)(**kw))
        waits = self._deps(q, reads, writes)
        self.dcnt[key] = self.dcnt.get(key, 0) + incval
        tok = ('d', key, self.dcnt[key])
        self.streams[q].append((waits, fn, ('c', key, incval)))
        self._upd(tok, reads, writes)
        return tok

    def barrier(self):
        toks = [('e', e, self.cnt[e]) for e in ENGS if self.cnt[e] > 0]
        toks += [('d', k, v) for k, v in self.dcnt.items() if not (isinstance(k, tuple) and k[0] == 'cc')]
        for eng in ENGS:
            waits = []
            for t in toks:
                k = (t[0], t[1])
                if self.waited[eng].get(k, 0) >= t[2]:
                    continue
                self.waited[eng][k] = t[2]
                waits.append(t)
            if waits:
                self.streams[eng].append((waits, None, None))
        self.res = {k: v for k, v in self.res.items() if isinstance(k, tuple) and k[0] == 'gx'}

    def emit(self):
        nc = self.nc
        with contextlib.ExitStack() as es:
            esem = {e: es.enter_context(nc.semaphore("s_" + e)) for e in ENGS}
            dsem = {k: es.enter_context(nc.semaphore("d_%d" % i)) for i, k in enumerate(self.dcnt)}
            block = es.enter_context(nc.Block())

            def run(name, h):
                for waits, fn, inc in self.streams[name]:
                    for kind, p, c in waits:
                        h.wait_ge(esem[p] if kind == 'e' else dsem[p], c)
                    if fn is None:
                        continue
                    ins = fn(h)
                    if inc[0] == 'e':
                        ins.then_inc(esem[inc[1]], 1)
                    elif inc[0] == 'd':
                        ins.then_inc(dsem[inc[1]], 16)
                    else:
                        ins.then_inc(dsem[inc[1]], inc[2])

            @block.tensor
            def _(h):
                run('pe', h)

            @block.scalar
            def _(h):
                run('act', h)

            @block.vector
            def _(h):
                run('dve', h)

            @block.gpsimd
            def _(h):
                run('pool', h)

            @block.sync
            def _(h):
                run('sp', h)


def build(nl, final_to_out=True, stop=None):
    nc = bass.Bass("TRN2", target_bir_lowering=False)
    P = Prog(nc)

    class _Stop(Exception):
        pass

    def din(name, shape, dt=F32):
        return nc.dram_tensor(name, list(shape), dt, kind="ExternalInput").ap()

    x_in = din("x", [TOK, D])
    w_in = din("w_in", [nl, D, INC])
    w_out = din("w_out", [nl, D, D])
    w_uq = din("w_uq", [nl, 384, 576])
    w_ukv = din("w_ukv", [nl, 256, 768])
    w_gu = din("w_gu", [nl, 16, D, 512])
    w_dn = din("w_dn", [nl, 16, 256, D])
    w_rt = din("w_rt", [D, 16])
    lnp = din("lnp", [nl, 4, 128, D])
    gnw = din("gnw", [nl, 128, 256])
    qnw = din("qnw", [nl, 128, 3])
    kvnw = din("kvnw", [nl, 128, 2])
    sinks = din("sinks", [nl, 1, 6])
    rbias = din("rbias", [128, 256])
    t_rcos = din("t_rcos", [128, NT * 32]); t_rsin = din("t_rsin", [128, NT * 32])
    t_cosq = din("t_cosq", [96, TOK]); t_sinq = din("t_sinq", [96, TOK])
    t_cosk = din("t_cosk", [32, TOK]); t_sink = din("t_sink", [32, TOK])
    t_decay = din("t_decay", [128, 512]); t_xit = din("t_xit", [128, 256]); t_zeta = din("t_zeta", [128, 256])
    t_bd = din("t_bd", [128, 256]); t_diagt = din("t_diagt", [128, 1280]); t_diags = din("t_diags", [128, 1280])
    t_swam = din("t_swam", [128, 5 * 384], BF16); t_mlam = din("t_mlam", [128, 512], BF16)
    t_ident = din("t_ident", [128, 128], BF16)
    t_sel = din("t_sel", [16, 16 * 128], BF16)
    y_out = nc.dram_tensor("y", [TOK, D], F32, kind="ExternalOutput").ap()

    CH = [160, 256, 128, 192, 192, 192, 192]
    ex = [[nc.dram_tensor("ex%d_%d" % (l, i), [r_, 2048], BF16).ap() for i, r_ in enumerate(CH)] for l in range(nl)]
    gx = [[nc.dram_tensor("gx%d_%d" % (l, i), [4 * r_, 2048], BF16).ap() for i, r_ in enumerate(CH)] for l in range(nl)]
    exu = [[nc.dram_tensor("exu%d_%d" % (l, i), [256, 1024], F32).ap() for i in range(2)] for l in range(nl)]
    gxu = [[nc.dram_tensor("gxu%d_%d" % (l, i), [1024, 1024], F32).ap() for i in range(2)] for l in range(nl)]
    xres = [nc.dram_tensor("xres%d" % l, [TOK, D], F32).ap() for l in range(nl)]

    es = contextlib.ExitStack()
    ARENA = 204 * 1024
    arena = es.enter_context(nc.sbuf_tensor("arena", [128, ARENA], U8))
    psb = [es.enter_context(nc.psum_tensor("ps%d" % k, [128, 512], F32)) for k in range(8)]

    def PS(k):
        return psb[k][:]

    def PSB(k):
        return psb[k][:].bitcast(BF16)

    class Mem:
        def __init__(self):
            self.top = 0

        def alloc(self, nbytes):
            off = self.top
            self.top = off + ((nbytes + 63) // 64) * 64
            assert self.top <= ARENA, ("SBUF overflow", self.top)
            return off

    mem = Mem()

    def V(off, dt, *dims):
        esz = 4 if dt == F32 else 2
        n = 1
        for d_ in dims:
            n *= d_
        ap = arena[:, off:off + n * esz].bitcast(dt)
        if len(dims) == 2:
            ap = ap.rearrange("p (a b) -> p a b", a=dims[0])
        elif len(dims) == 3:
            ap = ap.rearrange("p (a b c) -> p a b c", a=dims[0], b=dims[1])
        elif len(dims) == 4:
            ap = ap.rearrange("p (a b c d) -> p a b c d", a=dims[0], b=dims[1], c=dims[2])
        return ap

    OFFS = {}

    def T(nm, dt, *dims):
        esz = 4 if dt == F32 else 2
        n = 1
        for d_ in dims:
            n *= d_
        OFFS[nm] = mem.alloc(n * esz)
        return V(OFFS[nm], dt, *dims)

    ident = T('ident', BF16, 128)
    ones_bf = T('ones', BF16, 128)
    bufA = T('bufA', BF16, 8, TOK)
    bufB = T('bufB', BF16, 8, TOK)
    rbias_sb = T('rbias', F32, 256)
    wrt = T('wrt', BF16, 8, 16)
    sel_sb = T('sel', BF16, 16, 128)
    P.dma('sp', ident, t_ident, [], ['ident'], 'c0')
    P.dma('sp', rbias_sb, rbias, [], ['rbias'], 'c0')
    P.dma('sp', sel_sb[0:16], t_sel.rearrange("p (a b) -> p a b", a=16), [], ['sel'], 'c0')
    P.dma('pool', wrt, w_rt.rearrange("(c p) n -> p c n", p=128), [], ['wrt'], 'c1')
    P.op('dve', 'memset', dict(ap=ones_bf, constant=1.0), [], ['ones'])
    base_top = mem.top

    def transpose_to(src_bf, src_res, n, dst, dst_res, bank):
        pb = PSB(bank)
        for c in range(n):
            P.op('pe', 'transpose', dict(out=pb[:, c * 128:(c + 1) * 128], in_=src_bf[:, c * 128:(c + 1) * 128], identity=ident),
                 [src_res, 'ident'], [('ps', bank)])
        P.op('act', 'copy', dict(out=dst, in_=pb[:, 0:n * 128].rearrange("p (a b) -> p a b", a=n)),
             [('ps', bank)], [dst_res])

    def skew(stages, n):
        ns = len(stages)
        for step in range(n + ns - 1):
            for si in range(ns - 1, -1, -1):
                t = step - si
                if 0 <= t < n:
                    stages[si](t)


    xs = [T('xs%d' % k, F32, D) for k in range(2)]
    xb = [T('xb%d' % k, BF16, D) for k in range(2)]
    for t in range(NT):
        k = t % 2
        P.dma('sp', xs[k], x_in[t * 128:(t + 1) * 128, :], [], [('xs', k)], ('xs', k))
        P.op('dve', 'tensor_copy', dict(out=xb[k], in_=xs[k]), [('xs', k)], [('xb', k)])
        transpose_to(xb[k], ('xb', k), 8, bufA[:, :, t * 128:(t + 1) * 128], ('xT', t), t % 2)
    P.barrier()
    mem.top = base_top

    try:
      if stop == 'init':
          raise _Stop()
      for l in range(nl):
        last = (l == nl - 1)
        x_src = x_in if l == 0 else xres[l - 1]
        x_dst = y_out if (last and final_to_out) else xres[l]
        xT = bufA
        catT = bufB
        mem.top = base_top
        sqT = T('sqT', BF16, 3, TOK)
        cqnT = T('cqnT', BF16, 3, TOK)
        pers2_top = mem.top
        qT = T('qT', BF16, 2, TOK); qxiT = T('qxiT', BF16, 2, TOK); kT = T('kT', BF16, 2, TOK)
        v_tm = T('v_tm', BF16, NT, 256); sg_tm = T('sg_tm', BF16, NT, 256)
        pers_top = mem.top
        win = T('win', BF16, 8, 1184)
        wukv = T('wukv', BF16, 2, 768)
        wkrp = T('wkrp', BF16, 8, 32)
        kvg = T('kvg', F32, 2)
        rcos = T('rcos', F32, NT, 32); rsin = T('rsin', F32, NT, 32)
        _save = mem.top
        mem.top = OFFS['bufB']
        cosk = T('cosk', F32, TOK); sink_t = T('sink_t', F32, TOK)
        sv_st = T('sv_st', BF16, NT, 2, 192)
        assert mem.top <= OFFS['bufB'] + 8 * TOK * 2
        mem.top = _save
        decay = T('decay', F32, 512); xit = T('xit', F32, 2, 128); zeta = T('zeta', F32, 256); bd = T('bd', F32, 256)
        vm_st = [T('vm_st%d' % k, BF16, 384) for k in range(2)]
        sk_st = T('sk_st', BF16, TOK)
        kp_st = T('kp_st', BF16, TOK)
        ckvn = [T('ckvn%d' % k, BF16, 2, 512) for k in range(2)]
        xit_bf = T('xit_bf', BF16, 2, 128); zeta_bf = T('zeta_bf', BF16, 256)
        sq_sc = [T('sq%d' % k, BF16, 512) for k in range(3)]
        rstd = T('rstd', F32, 512)
        kn_st = [T('kn%d' % k, BF16, 512) for k in range(4)]
        rt = [T('rt%d' % k, F32, 256) for k in range(4)]
        qk_tm = [T('qk_tm%d' % k, BF16, 512) for k in range(2)]
        kz = [T('kz%d' % k, BF16, 256) for k in range(2)]
        u_st = [T('u_st%d' % k, F32, 256) for k in range(2)]
        kpt = [T('kpt%d' % k, F32, 512) for k in range(2)]

        for c in range(8):
            P.dma('pool', win[:, c, 0:1024], w_in[l, c * 128:(c + 1) * 128, 0:1024], [], ['win'], 'win')
            P.dma('pool', win[:, c, 1024:1152], w_in[l, c * 128:(c + 1) * 128, 1536:1664], [], ['win'], 'win')
        P.dma('pool', wukv, w_ukv[l].rearrange("(c p) n -> p c n", p=128), [], ['wukv'], 'win')
        P.dma('sp', kvg, kvnw[l], [], ['kvg'], 'tabA')
        for dst, src in ((rcos, t_rcos), (rsin, t_rsin)):
            P.dma('sp', dst, src.rearrange("p (a b) -> p a b", a=NT), [], ['tabA_'], 'tabA')
        P.dma('sp', cosk[0:32], t_cosk, [], ['tabA_'], 'tabA')
        P.dma('sp', sink_t[0:32], t_sink, [], ['tabA_'], 'tabA')
        P.dma('sp', decay, t_decay, [], ['tabA_'], 'tabA')
        P.dma('sp', xit, t_xit.rearrange("p (a b) -> p a b", a=2), [], ['tabA_'], 'tabA')
        P.dma('sp', zeta, t_zeta, [], ['tabA_'], 'tabA')
        P.dma('sp', bd, t_bd, [], ['tabA_'], 'tabA')
        P.op('dve', 'memset', dict(ap=sv_st, constant=1.0), [], ['sv_st'])
        P.op('dve', 'tensor_copy', dict(out=xit_bf, in_=xit), ['tabA_'], ['xit_bf'])
        P.op('dve', 'tensor_copy', dict(out=zeta_bf, in_=zeta), ['tabA_'], ['zeta_bf'])
        for kc in range(2):
            P.op('dve', 'tensor_scalar', dict(out=wukv[:, kc, :], in0=wukv[:, kc, :], scalar1=kvg[:, kc:kc + 1],
                                                         scalar2=None, op0=ALU.mult), ['wukv', 'kvg'], ['wukv'])

        if stop == 'A0':
            raise _Stop()
        def tm0(t):
            tc_ = slice(t * 128, (t + 1) * 128)
            k2 = t % 2
            ba, bb, bc = 0 + k2, 2 + k2, 4
            for (bank, c0, n) in ((ba, 0, 512), (bb, 512, 512), (bc, 1024, 128)):
                for c in range(8):
                    P.op('pe', 'matmul', dict(out=PS(bank)[:, 0:n], lhsT=xT[:, c, tc_], rhs=win[:, c, c0:c0 + n], start=(c == 0), stop=(c == 7)),
                         [('xT', t), 'win'], [('ps', bank)])

        def tm1(t):
            k2 = t % 2
            ba, bb, bc = 0 + k2, 2 + k2, 4
            pa = PS(ba).rearrange("p (h d) -> p h d", h=8)
            x1, x2 = pa[:, :, 0:32], pa[:, :, 32:64]
            cb = rcos[:, t, :].unsqueeze(1).broadcast_to([128, 8, 32])
            sb_ = rsin[:, t, :].unsqueeze(1).broadcast_to([128, 8, 32])
            r0, r1, r2, r3 = [rt[i].rearrange("p (h d) -> p h d", h=8) for i in range(4)]
            P.op('dve', 'tensor_tensor', dict(out=r0, in0=x1, in1=cb, op=ALU.mult), [('ps', ba), 'tabA_'], ['rt0'])
            P.op('dve', 'tensor_tensor', dict(out=r1, in0=x2, in1=sb_, op=ALU.mult), [('ps', ba)], ['rt1'])
            P.op('dve', 'tensor_tensor', dict(out=r2, in0=x1, in1=sb_, op=ALU.mult), [('ps', ba)], ['rt2'])
            P.op('dve', 'tensor_tensor', dict(out=r3, in0=x2, in1=cb, op=ALU.mult), [('ps', ba)], ['rt3'])
            P.op('act', 'copy', dict(out=v_tm[:, t, :], in_=PS(bb)[:, 0:256]), [('ps', bb)], [('v_tm', t)])
            P.op('act', 'activation', dict(out=sg_tm[:, t, :], in_=PS(bb)[:, 256:512], func=AF.Silu), [('ps', bb)], [('sg_tm', t)])
            P.op('act', 'copy', dict(out=sv_st[:, t, :, 64:128], in_=PS(bc)[:, 0:128].rearrange("p (a b) -> p a b", a=2)), [('ps', bc)], ['sv_st'])

        def tm2(t):
            k2 = t % 2
            r0, r1, r2, r3 = [rt[i].rearrange("p (h d) -> p h d", h=8) for i in range(4)]
            qk3 = qk_tm[k2].rearrange("p (h d) -> p h d", h=8)
            P.op('pool', 'tensor_tensor', dict(out=qk3[:, :, 0:32], in0=r0, in1=r1, op=ALU.subtract), ['rt0', 'rt1'], [('qk_tm', k2)])
            P.op('pool', 'tensor_tensor', dict(out=qk3[:, :, 32:64], in0=r2, in1=r3, op=ALU.add), ['rt2', 'rt3', ('qk_tm', k2)], [('qk_tm', k2)])

        def tm3(t):
            tc_ = slice(t * 128, (t + 1) * 128)
            k2 = t % 2
            bt = 6 + k2
            pb = PSB(bt)
            for c in range(4):
                P.op('pe', 'transpose', dict(out=pb[:, c * 128:(c + 1) * 128], in_=qk_tm[k2][:, c * 128:(c + 1) * 128], identity=ident),
                     [('qk_tm', k2), 'ident'], [('ps', bt)])
            pb3 = pb[:, 0:512].rearrange("p (a b) -> p a b", a=4)
            P.op('act', 'copy', dict(out=qT[:, :, tc_], in_=pb3[:, 0:2, :]), [('ps', bt)], [('qT', t)])
            P.op('act', 'copy', dict(out=kT[:, :, tc_], in_=pb3[:, 2:4, :]), [('ps', bt)], [('kT', t)])
            P.op('pool', 'tensor_tensor', dict(out=kz[k2], in0=qk_tm[k2][:, 256:512], in1=zeta_bf, op=ALU.mult), [('qk_tm', k2), 'zeta_bf'], [('kz', k2)])

        def tm4(t):
            tc_ = slice(t * 128, (t + 1) * 128)
            k2 = t % 2
            bu = 5
            P.op('dve', 'tensor_tensor', dict(out=qxiT[:, :, tc_], in0=qT[:, :, tc_], in1=xit_bf, op=ALU.mult), [('qT', t), 'xit_bf'], [('qxiT', t)])
            for c in range(2):
                P.op('pe', 'matmul', dict(out=PS(bu)[:, c * 128:128 + c * 128], lhsT=kz[k2][:, c * 128:(c + 1) * 128],
                                          rhs=v_tm[:, t, c * 128:(c + 1) * 128], start=True, stop=True),
                     [('kz', k2), ('v_tm', t)], [('ps', bu)])
            P.op('dve', 'tensor_tensor', dict(out=u_st[k2], in0=PS(bu)[:, 0:256], in1=bd, op=ALU.mult), [('ps', bu), 'tabA_'], [('u_st', k2)])
            P.dma('sp', exu[l][t // 8][(t % 8) * 32:(t % 8 + 1) * 32, :].rearrange("a b -> (a b)").rearrange("(p x) -> p x", p=128), u_st[k2],
                  [('u_st', k2)], [('exw', 'u', t // 8)], ('exw', 'u', t // 8))

        skew([tm0, tm1, tm2, tm3, tm4], NT)

        if stop == 'A1':
            raise _Stop()
        for c in range(8):
            P.dma('pool', win[:, c, 0:512], w_in[l, c * 128:(c + 1) * 128, 1024:1536], [], ['win'], 'win')
            P.dma('pool', win[:, c, 512:1184], w_in[l, c * 128:(c + 1) * 128, 1664:2336], [], ['win'], 'win')
        P.op('dve', 'tensor_scalar', dict(out=wkrp[:, :, 0:16], in0=win[:, :, 1168:1184], scalar1=-1.0, scalar2=None,
                                          op0=ALU.mult), ['win'], ['wkrp'])
        P.op('dve', 'tensor_copy', dict(out=wkrp[:, :, 16:32], in_=win[:, :, 1152:1168]), ['win', 'wkrp'], ['wkrp'])
        rg = [[0, 1, 2, 3], [4, 5, 6, 7]]
        for hf in range(2):
            P.dma('sp', ex[l][3 + hf].rearrange("a b -> (a b)").rearrange("(t p x) -> p t x", t=8, p=128),
                  sv_st[:, hf * 8:(hf + 1) * 8].rearrange("p t k x -> p t (k x)"), ['sv_st'], [('exw', 3 + hf)], ('exw', 3 + hf))
        for ci in range(2):
            P.custom('pool', 'collective_compute', dict(kind="AllGather", op=ALU.bypass, replica_groups=rg, ins=[exu[l][ci].opt()], outs=[gxu[l][ci].opt()]),
                 [('exw', 'u', ci)], [('gx', 'u', ci)], ('cc', l, 'u', ci), 1)
        for ci in (3, 4):
            P.custom('pool', 'collective_compute', dict(kind="AllGather", op=ALU.bypass, replica_groups=rg, ins=[ex[l][ci].opt()], outs=[gx[l][ci].opt()]),
                 [('exw', ci)], [('gx', ci)], ('cc', l, ci), 1)
        def fmm(G, bank, c0, m, extra_w=None):
            gc = slice(G * 512, (G + 1) * 512)
            for c in range(8):
                wsrc = (win[:, c, c0:c0 + m] if extra_w is None else extra_w[:, c, :])
                P.op('pe', 'matmul', dict(out=PS(bank)[0:m, :], lhsT=wsrc, rhs=xT[:, c, gc], start=(c == 0), stop=(c == 7)),
                     ['win', 'wkrp'], [('ps', bank)])

        def kv0(G):
            gc = slice(G * 512, (G + 1) * 512)
            cb = 6 if G % 2 == 0 else 4
            for c2 in range(2):
                fmm(G, cb + c2, 896 + c2 * 128, 128)
                P.op('act', 'activation', dict(out=sq_sc[c2], in_=PS(cb + c2), func=AF.Square), [('ps', cb + c2)], [('sq', c2)])
            fmm(G, 0, 384, 128)
            P.op('act', 'copy', dict(out=sk_st[:, gc], in_=PS(0)), [('ps', 0)], ['sk_st'])
            fmm(G, 1, 1152, 32)
            fmm(G, 2, 0, 32, extra_w=wkrp)
            kp0, kp1 = kpt[0], kpt[1]
            P.op('dve', 'tensor_tensor', dict(out=kp0[0:32], in0=PS(1)[0:32], in1=cosk[0:32, gc], op=ALU.mult), [('ps', 1), 'tabA_'], ['kp0'])
            P.op('dve', 'tensor_tensor', dict(out=kp1[0:32], in0=PS(2)[0:32], in1=sink_t[0:32, gc], op=ALU.mult), [('ps', 2), 'tabA_'], ['kp1'])
            P.op('dve', 'tensor_tensor', dict(out=kp_st[0:32, gc], in0=kp0[0:32], in1=kp1[0:32], op=ALU.add), ['kp0', 'kp1'], ['kp_st'])

        def kv1(G):
            cb = 6 if G % 2 == 0 else 4
            ck = ckvn[G % 2]
            for c2 in range(2):
                P.op('pe', 'matmul', dict(out=PS(3), lhsT=ones_bf, rhs=sq_sc[c2], start=(c2 == 0), stop=(c2 == 1)), [('sq', c2), 'ones'], [('ps', 3)])
            P.op('act', 'activation', dict(out=rstd, in_=PS(3), func=AF.Sqrt, scale=1.0 / 256.0, bias=RMS_EPS), [('ps', 3)], ['rstd'])
            P.op('dve', 'reciprocal', dict(out=rstd, in_=rstd), ['rstd'], ['rstd'])
            for c2 in range(2):
                P.op('dve', 'tensor_tensor', dict(out=ck[:, c2, :], in0=PS(cb + c2), in1=rstd, op=ALU.mult), [('ps', cb + c2), 'rstd'], [('ckvn', G % 2)])

        def kv2(G):
            gc = slice(G * 512, (G + 1) * 512)
            cb = 6 if G % 2 == 0 else 4
            ck = ckvn[G % 2]
            nb = 0
            for h in range(6):
                bank = cb + (nb % 2)
                nb += 1
                ks = kn_st[h % 4]
                for kc in range(2):
                    P.op('pe', 'matmul', dict(out=PS(bank)[0:64, :], lhsT=wukv[:, kc, h * 128:h * 128 + 64], rhs=ck[:, kc, :], start=(kc == 0), stop=(kc == 1)),
                         [('ckvn', G % 2), 'wukv'], [('ps', bank)])
                P.op('act', 'copy', dict(out=ks[0:64], in_=PS(bank)[0:64, :]), [('ps', bank)], [('kn', h % 4)])
                P.dma('sp', ex[l][1 + h // 4][(h % 4) * 64:(h % 4 + 1) * 64, gc], ks[0:64], [('kn', h % 4)], [('exw', 1 + h // 4)], ('exw', 1 + h // 4))
            vv = wukv.rearrange("p c (h x) -> p c h x", h=6)
            for tt in range(4):
                t = G * 4 + tt
                bank = cb + (nb % 2)
                nb += 1
                for kc in range(2):
                    P.op('pe', 'matmul', dict(out=PS(bank)[:, 0:384].rearrange("p (h x) -> p h x", h=6), lhsT=ck[:, kc, tt * 128:(tt + 1) * 128],
                                              rhs=vv[:, kc, :, 64:128], start=(kc == 0), stop=(kc == 1)), [('ckvn', G % 2), 'wukv'], [('ps', bank)])
                P.op('act', 'copy', dict(out=vm_st[tt % 2], in_=PS(bank)[:, 0:384]), [('ps', bank)], [('vm_st', tt % 2)])
                P.dma('sp', ex[l][5 + t // 8][(t % 8) * 24:(t % 8 + 1) * 24, :].rearrange("a b -> (a b)").rearrange("(p x) -> p x", p=128), vm_st[tt % 2],
                      [('vm_st', tt % 2)], [('exw', 5 + t // 8)], ('exw', 5 + t // 8))

        skew([kv0, kv1, kv2], 4)
        P.dma('sp', ex[l][0][0:128, :], sk_st, ['sk_st'], [('exw', 0)], ('exw', 0))
        P.dma('sp', ex[l][0][128:160, :], kp_st[0:32], ['kp_st'], [('exw', 0)], ('exw', 0))
        for ci in (0, 1, 2, 5, 6):
            P.custom('pool', 'collective_compute', dict(kind="AllGather", op=ALU.bypass, replica_groups=rg, ins=[ex[l][ci].opt()], outs=[gx[l][ci].opt()]),
                 [('exw', ci)], [('gx', ci)], ('cc', l, ci), 1)
        def q0(G):
            gc = slice(G * 512, (G + 1) * 512)
            cb = 2 if G % 2 == 0 else 5
            for c3 in range(3):
                fmm(G, cb + c3, 512 + c3 * 128, 128)
                P.op('act', 'activation', dict(out=sq_sc[c3], in_=PS(cb + c3), func=AF.Square), [('ps', cb + c3)], [('sq', c3)])
            for c3 in range(3):
                fmm(G, 0, c3 * 128, 128)
                P.op('act', 'mul', dict(out=sqT[:, c3, gc], in_=PS(0), mul=0.125), [('ps', 0)], [('sqT', G)])

        def q1(G):
            gc = slice(G * 512, (G + 1) * 512)
            cb = 2 if G % 2 == 0 else 5
            for c3 in range(3):
                P.op('pe', 'matmul', dict(out=PS(1), lhsT=ones_bf, rhs=sq_sc[c3], start=(c3 == 0), stop=(c3 == 2)), [('sq', c3), 'ones'], [('ps', 1)])
            P.op('act', 'activation', dict(out=rstd, in_=PS(1), func=AF.Sqrt, scale=1.0 / 384.0, bias=RMS_EPS), [('ps', 1)], ['rstd'])
            P.op('dve', 'reciprocal', dict(out=rstd, in_=rstd), ['rstd'], ['rstd'])
            for c3 in range(3):
                P.op('dve', 'tensor_tensor', dict(out=cqnT[:, c3, gc], in0=PS(cb + c3), in1=rstd, op=ALU.mult), [('ps', cb + c3), 'rstd'], [('cqnT', G)])

        skew([q0, q1], 4)
        P.barrier()
        if stop == 'A' or stop == 'X':
            raise _Stop()
        gxl = gx[l]

        if stop == 'X':
            raise _Stop()
        mem.top = pers_top
        diagt = T('diagt', F32, 2, 5, 128); diags = T('diags', F32, 2, 5, 128)
        gn_sb = T('gn_sb', F32, 256)
        decay = T('decay', F32, 512)
        Tst = T('Tst', F32, 2, 128)
        Sbd = T('Sbd', BF16, NT, 2, 128)
        ug = [T('ug%d' % k, F32, 4, 2, 128) for k in range(2)]
        sacc = T('sacc', F32, 2, 128)
        sTm = [T('sTm%d' % k, BF16, 512) for k in range(2)]
        bst = [T('bst%d' % k, F32, 4, 6) for k in range(2)]; mv = [T('mv%d' % k, F32, 4, 2) for k in range(2)]; rs4 = [T('rs4%d' % k, F32, 4) for k in range(2)]
        yb = [T('yb%d' % k, F32, 256) for k in range(2)]
        yb2 = [T('yb2%d' % k, F32, 256) for k in range(2)]
        ro = [T('ro%d' % k, BF16, 256) for k in range(2)]
        P.dma('sp', diagt, t_diagt.rearrange("p (a b c) -> p a b c", a=2, b=5), [], ['tabB'], 'tabB')
        P.dma('sp', diags, t_diags.rearrange("p (a b c) -> p a b c", a=2, b=5), [], ['tabB'], 'tabB')
        P.dma('sp', gn_sb, gnw[l], [], ['tabB'], 'tabB')
        P.dma('sp', decay, t_decay, [], ['tabB'], 'tabB')
        P.op('dve', 'memset', dict(ap=Tst, constant=0.0), [], ['Tst'])
        def b1_rec(i):
            k2 = i % 2
            for r in range(4):
                P.dma('sp', ug[k2][:, r].rearrange("p c x -> p (c x)"),
                      gxu[l][i // 8][r * 256 + (i % 8) * 32:r * 256 + (i % 8 + 1) * 32, :].rearrange("a b -> (a b)").rearrange("(p x) -> p x", p=128),
                      [('gx', 'u', i // 8)], [('ug', k2)], ('ug', k2))
            for (dg, bank) in ((diags, 6), (diagt, 7)):
                for c in range(2):
                    for sidx in range(5):
                        rhs = Tst[:, c, :] if sidx == 0 else ug[k2][:, sidx - 1, c, :]
                        P.op('pe', 'matmul', dict(out=PS(bank)[:, c * 128:(c + 1) * 128], lhsT=dg[:, c, sidx, :], rhs=rhs, start=(sidx == 0), stop=(sidx == 4)),
                             ['Tst', ('ug', k2), 'tabB'], [('ps', bank)])
            P.op('act', 'copy', dict(out=Sbd[:, i].rearrange("p c x -> p (c x)"), in_=PS(6)[:, 0:256]), [('ps', 6)], [('Sbd', i)])
            P.op('act', 'copy', dict(out=Tst.rearrange("p c x -> p (c x)"), in_=PS(7)[:, 0:256]), [('ps', 7)], ['Tst'])

        def b1_s0(t):
            tc_ = slice(t * 128, (t + 1) * 128)
            k2 = t % 2
            bs = 0 + k2
            b1_rec(t)
            for h in range(4):
                rows = slice((h % 2) * 64, (h % 2) * 64 + 64)
                P.op('pe', 'matmul', dict(out=PS(bs)[:, h * 128:(h + 1) * 128], lhsT=kT[rows, h // 2, tc_], rhs=qT[rows, h // 2, tc_], start=True, stop=True),
                     [('kT', t), ('qT', t)], [('ps', bs)])
            P.op('dve', 'tensor_tensor', dict(out=sTm[k2], in0=PS(bs), in1=decay, op=ALU.mult), [('ps', bs), 'tabB'], [('sTm', k2)])

        def b1_s1(t):
            tc_ = slice(t * 128, (t + 1) * 128)
            k2 = t % 2
            bo = 2 + k2
            for c in range(2):
                P.op('pe', 'matmul', dict(out=PS(bo)[:, c * 128:(c + 1) * 128], lhsT=qxiT[:, c, tc_], rhs=Sbd[:, t, c, :], start=True, stop=False),
                     [('qxiT', t), ('Sbd', t)], [('ps', bo)])
                for hh in range(2):
                    h = 2 * c + hh
                    P.op('pe', 'matmul', dict(out=PS(bo)[:, h * 64:(h + 1) * 64], lhsT=sTm[k2][:, h * 128:(h + 1) * 128],
                                              rhs=v_tm[:, t, h * 64:(h + 1) * 64], start=False, stop=(hh == 1)),
                         [('sTm', k2), ('v_tm', t)], [('ps', bo)])

        def b1_s2(t):
            k2 = t % 2
            bo = 2 + k2
            for h in range(4):
                P.op('dve', 'bn_stats', dict(out=bst[k2][:, h, :], in_=PS(bo)[:, h * 64:(h + 1) * 64]), [('ps', bo)], [('bst', k2)])
            for h in range(4):
                P.op('dve', 'bn_aggr', dict(out=mv[k2][:, h, :], in_=bst[k2][:, h, :]), [('bst', k2)], [('mv', k2)])
            P.op('act', 'activation', dict(out=rs4[k2], in_=mv[k2][:, :, 1], func=AF.Sqrt, bias=LN_EPS), [('mv', k2)], [('rs4', k2)])

        def b1_s3(t):
            k2 = t % 2
            bo = 2 + k2
            P.op('dve', 'reciprocal', dict(out=rs4[k2], in_=rs4[k2]), [('rs4', k2)], [('rs4', k2)])
            for h in range(4):
                P.op('dve', 'tensor_scalar', dict(out=yb[k2][:, h * 64:(h + 1) * 64], in0=PS(bo)[:, h * 64:(h + 1) * 64],
                                                  scalar1=mv[k2][:, h, 0:1], scalar2=rs4[k2][:, h:h + 1], op0=ALU.subtract, op1=ALU.mult),
                     [('ps', bo), ('mv', k2), ('rs4', k2)], [('yb', k2)])
            P.op('dve', 'tensor_tensor', dict(out=yb2[k2], in0=yb[k2], in1=gn_sb, op=ALU.mult), [('yb', k2), 'tabB'], [('yb2', k2)])
            P.op('act', 'copy', dict(out=yb[k2], in_=sg_tm[:, t, :]), [('sg_tm', t), ('yb', k2), ('yb2', k2)], [('yb', k2)])
            P.op('dve', 'tensor_tensor', dict(out=ro[k2], in0=yb2[k2], in1=yb[k2], op=ALU.mult), [('yb2', k2), ('yb', k2)], [('ro', k2)])

        def b1_s4(t):
            tc_ = slice(t * 128, (t + 1) * 128)
            k2 = t % 2
            transpose_to(ro[k2], ('ro', k2), 2, catT[:, 0:2, tc_], ('catT', t), 4 + k2)

        skew([b1_s0, b1_s1, b1_s2, b1_s3, b1_s4], NT)
        P.barrier()

        if stop == 'B1':
            raise _Stop()
        mem.top = pers2_top
        skt = T('skt', BF16, 2, 4, TOK)
        svg = T('svg', BF16, 4, NT, 384)
        swam = T('swam', BF16, 5, 384)
        esr = T('esr', BF16, 6, 128)
        esf = T('esf', F32, 8)
        onesv = T('onesv', BF16, 2, 128)
        pt = [T('pt%d' % k, BF16, 384) for k in range(6)]
        ptm = [T('ptm%d' % k, BF16, 384) for k in range(6)]
        rec = [T('rec%d' % k, F32, 384) for k in range(2)]
        for r in range(4):
            base = r * 160
            P.dma('sp', skt[:, 0, r, :], gxl[0][base:base + 128, :], [('gx', 0)], ['skt'], 'tabC')
            P.dma('sp', skt[0:64, 1, r, :], gxl[0][base + 64:base + 128, :], [('gx', 0)], ['skt'], 'tabC')
            P.dma('sp', skt[64:128, 1, r, :], gxl[0][base:base + 64, :], [('gx', 0)], ['skt'], 'tabC')
            for hf in range(2):
                P.dma('sp', svg[:, r, hf * 8:(hf + 1) * 8], gxl[3 + hf][r * 192:(r + 1) * 192, :].rearrange("a b -> (a b)").rearrange("(t p x) -> p t x", t=8, p=128),
                      [('gx', 3 + hf)], ['svg'], 'tabC')
        P.dma('sp', swam, t_swam.rearrange("p (a b) -> p a b", a=5), [], ['swam'], 'tabC')
        P.dma('sp', esf[0:1, 0:6], sinks[l], [], ['esf'], 'tabC')
        P.op('act', 'activation', dict(out=esf[0:1, 0:6], in_=esf[0:1, 0:6], func=AF.Exp), ['esf'], ['esf'])
        P.op('dve', 'tensor_copy', dict(out=esr[0:1], in_=esf[0:1, 0:6].unsqueeze(2).broadcast_to([1, 6, 128])), ['esf'], ['esr'])
        P.op('dve', 'memset', dict(ap=onesv[0:1], constant=0.0), [], ['onesv'])
        P.op('dve', 'memset', dict(ap=onesv[0:1, 0, 64:128], constant=1.0), ['onesv'], ['onesv'])
        P.op('dve', 'memset', dict(ap=onesv[0:1, 1, 0:64], constant=1.0), ['onesv'], ['onesv'])
        svg5 = svg.rearrange("p r t (k x) -> p r t k x", k=2)
        LA2 = 3
        nu = 0
        for t in range(NT):
            tc_ = slice(t * 128, (t + 1) * 128)
            cands = [(0, t), (1, t), (2, t), (3, t)] + ([(3, t - 1)] if t > 0 else [])
            bo = [4 + (t % 2) * 2, 5 + (t % 2) * 2]
            units = [(ci, par) for ci in range(len(cands)) for par in range(2)]
            NU = len(units)
            uinfo = {}

            def qk2(u):
                nonlocal nu
                ci, par = units[u]
                r, il = cands[ci]
                midx = ci if ci < 4 else 4
                kc_ = slice(il * 128, (il + 1) * 128)
                slot = nu % 6
                bank = nu % 4
                nu += 1
                uinfo[u] = slot
                o = par * 64
                for g in range(3):
                    hq = 2 * g + par
                    hk = hq // 3
                    var = 0 if (o == 0) == (hk == 0) else 1
                    P.op('pe', 'matmul', dict(out=PS(bank)[:, g * 128:(g + 1) * 128], lhsT=skt[o:o + 64, var, r, kc_], rhs=sqT[o:o + 64, hq // 2, tc_],
                                              start=True, stop=True), ['skt', ('sqT', t // 4)], [('ps', bank)])
                P.op('act', 'activation', dict(out=pt[slot], in_=PS(bank)[:, 0:384], func=AF.Exp), [('ps', bank)], [('pt', slot)])
                P.op('pool', 'tensor_tensor', dict(out=ptm[slot], in0=pt[slot], in1=swam[:, midx, :], op=ALU.mult),
                     [('pt', slot), 'swam'], [('ptm', slot)])

            def pv2(u):
                ci, par = units[u]
                r, il = cands[ci]
                slot = uinfo[u]
                for g in range(3):
                    hq = 2 * g + par
                    hk = hq // 3
                    lw = svg5[:, r, il, hk, 64:192] if par == 0 else svg5[:, r, il, hk, 0:128]
                    P.op('pe', 'matmul', dict(out=PS(bo[par])[:, g * 128:(g + 1) * 128], lhsT=lw, rhs=ptm[slot][:, g * 128:(g + 1) * 128],
                                              start=(ci == 0 and g == 0), stop=False), ['svg', ('ptm', slot)], [('ps', bo[par])])

            for u in range(NU + LA2):
                if u < NU:
                    qk2(u)
                if u - LA2 >= 0:
                    pv2(u - LA2)
            for hq in range(6):
                par = hq % 2
                col = (hq // 2) * 128
                P.op('pe', 'matmul', dict(out=PS(bo[par])[:, col:col + 128], lhsT=onesv[0:1, par, :], rhs=esr[0:1, hq, :], start=False, stop=(hq >= 4)),
                     ['onesv', 'esr'], [('ps', bo[par])])
            for par in range(2):
                orow = slice(par * 64, par * 64 + 64)
                drow = slice((1 - par) * 64, (1 - par) * 64 + 64)
                P.op('dve', 'reciprocal', dict(out=rec[par][orow], in_=PS(bo[par])[drow, 0:384]), [('ps', bo[par])], [('rec', par)])
                P.op('dve', 'tensor_tensor', dict(out=catT[orow, 2:5, tc_], in0=PS(bo[par])[orow, 0:384].rearrange("p (a b) -> p a b", a=3),
                                                  in1=rec[par][orow].rearrange("p (a b) -> p a b", a=3), op=ALU.mult),
                     [('ps', bo[par]), ('rec', par)], [('catT2', t)])
        P.barrier()

        if stop == 'B2':
            raise _Stop()
        mem.top = pers2_top
        wuq = T('wuq', BF16, 3, 576); wuqp = T('wuqp', BF16, 3, 576)
        qg = T('qg', F32, 3)
        cosq = T('cosq', F32, TOK); sinq = T('sinq', F32, TOK)
        mlam = T('mlam', BF16, 4, 128)
        _save = mem.top
        mem.top = OFFS['bufA']
        kth = [T('kth%d' % k, BF16, 4, TOK) for k in range(2)]
        assert mem.top <= OFFS['bufA'] + 8 * TOK * 2
        mem.top = _save
        vh = [T('vh%d' % k, BF16, 4, NT, 128) for k in range(2)]
        qTh = [T('qTh%d' % k, BF16, TOK) for k in range(2)]
        q1 = [T('q1%d' % k, F32, 512) for k in range(2)]
        q2 = [T('q2%d' % k, F32, 512) for k in range(2)]
        NPT = 6
        ptl = [T('ptl%d' % k, BF16, 512) for k in range(NPT)]
        recm = [T('recm%d' % k, F32, 512) for k in range(2)]
        P.dma('pool', wuq, w_uq[l].rearrange("(c p) n -> p c n", p=128), [], ['wuq'], 'win')
        P.dma('sp', qg, qnw[l], [], ['qg'], 'tabD')
        P.dma('sp', cosq[0:96], t_cosq, [], ['tabD_'], 'tabD')
        P.dma('sp', sinq[0:96], t_sinq, [], ['tabD_'], 'tabD')
        P.dma('sp', mlam, t_mlam.rearrange("p (a b) -> p a b", a=4), [], ['mlam'], 'tabD')
        for kc in range(3):
            P.op('dve', 'tensor_scalar', dict(out=wuq[:, kc, :], in0=wuq[:, kc, :], scalar1=qg[:, kc:kc + 1], scalar2=None, op0=ALU.mult),
                 ['wuq', 'qg'], ['wuq'])
        P.op('dve', 'memset', dict(ap=wuqp, constant=0.0), [], ['wuqp'])
        w4 = wuq.rearrange("p c (h x) -> p c h x", h=6)
        wp4 = wuqp.rearrange("p c (h x) -> p c h x", h=6)
        for kc in range(3):
            P.op('dve', 'tensor_scalar', dict(out=wp4[:, kc, :, 64:80], in0=w4[:, kc, :, 80:96], scalar1=-1.0, scalar2=None, op0=ALU.mult),
                 ['wuq', 'wuqp'], ['wuqp'])
            P.op('dve', 'tensor_copy', dict(out=wp4[:, kc, :, 80:96], in_=w4[:, kc, :, 64:80]), ['wuq', 'wuqp'], ['wuqp'])
        P.op('dve', 'memset', dict(ap=vh[0][:, :, :, 64:128], constant=1.0), [], [('vh', 0)])
        P.op('dve', 'memset', dict(ap=vh[1][:, :, :, 0:64], constant=1.0), [], [('vh', 1)])
        for k in range(2):
            for r in range(4):
                P.dma('sp', kth[k][64:96, r, :], gxl[0][r * 160 + 128:r * 160 + 160, :], [('gx', 0)], [('kth', k)], ('kth', k))
        def mla_loads(h):
            k2 = h % 2
            for r in range(4):
                rows_ = CH[1 + h // 4]
                kb = r * rows_ + (h % 4) * 64
                P.dma('sp', kth[k2][0:64, r, :], gxl[1 + h // 4][kb:kb + 64, :], [('gx', 1 + h // 4)], [('kth', k2)], ('kth', k2))
                vcols = slice(0, 64) if k2 == 0 else slice(64, 128)
                for half in range(2):
                    vsrc = gxl[5 + half][r * 192:(r + 1) * 192, :].rearrange("a b -> (a b)").rearrange("(t p x) -> p t x", t=8, p=128)[:, :, h * 64:(h + 1) * 64]
                    P.dma('sp', vh[k2][:, r, half * 8:(half + 1) * 8, vcols], vsrc, [('gx', 5 + half)], [('vh', k2)], ('vh', k2))

        def mla_q(h):
            k2 = h % 2
            for G in range(4):
                gc = slice(G * 512, (G + 1) * 512)
                b1, b2 = 6, 7
                for kc in range(3):
                    P.op('pe', 'matmul', dict(out=PS(b1)[0:96, :], lhsT=wuq[:, kc, h * 96:(h + 1) * 96], rhs=cqnT[:, kc, gc],
                                              start=(kc == 0), stop=(kc == 2)), ['wuq', ('cqnT', G)], [('ps', b1)])
                for kc in range(3):
                    P.op('pe', 'matmul', dict(out=PS(b2)[0:96, :], lhsT=wuqp[:, kc, h * 96:(h + 1) * 96], rhs=cqnT[:, kc, gc],
                                              start=(kc == 0), stop=(kc == 2)), ['wuqp', ('cqnT', G)], [('ps', b2)])
                g2 = G % 2
                P.op('dve', 'tensor_tensor', dict(out=q1[g2][0:96], in0=PS(b1)[0:96, :], in1=cosq[0:96, gc], op=ALU.mult),
                     [('ps', b1), 'tabD_'], [('q1', g2)])
                P.op('dve', 'tensor_tensor', dict(out=q2[g2][0:96], in0=PS(b2)[0:96, :], in1=sinq[0:96, gc], op=ALU.mult),
                     [('ps', b2), 'tabD_'], [('q2', g2)])
                P.op('pool', 'tensor_tensor', dict(out=qTh[k2][0:96, gc], in0=q1[g2][0:96], in1=q2[g2][0:96], op=ALU.add),
                     [('q1', g2), ('q2', g2)], [('qTh', k2, G)])

        LA = 3
        mla_loads(0)
        mla_q(0)
        nkt = 0
        for h in range(6):
            k2 = h % 2
            if h + 1 < 6:
                mla_loads(h + 1)
                mla_q(h + 1)
            par = h % 2
            orow = slice(par * 64, par * 64 + 64)
            drow = slice((1 - par) * 64, (1 - par) * 64 + 64)
            blocks = [(G, il, r) for G in range(4) for il in range(4 * G + 4) for r in range(4)]
            NB = len(blocks)
            info = {}

            def qk(n):
                nonlocal nkt
                G, il, r = blocks[n]
                gc0 = G * 512
                a = max(0, il - 4 * G)
                qs = slice(gc0 + a * 128, gc0 + 512)
                cs = slice(a * 128, 512)
                bank = nkt % 4
                slot = nkt % NPT
                nkt += 1
                info[n] = (cs, slot)
                P.op('pe', 'matmul', dict(out=PS(bank)[:, cs], lhsT=kth[k2][0:96, r, il * 128:(il + 1) * 128], rhs=qTh[k2][0:96, qs],
                                          start=True, stop=True), [('kth', k2), ('qTh', k2, G)], [('ps', bank)])
                P.op('act', 'activation', dict(out=ptl[slot][:, cs], in_=PS(bank)[:, cs], func=AF.Exp), [('ps', bank)], [('ptl', slot)])
                if il >= 4 * G:
                    ms = slice(a * 128, a * 128 + 128)
                    P.op('dve', 'tensor_tensor', dict(out=ptl[slot][:, ms], in0=ptl[slot][:, ms], in1=mlam[:, r, :], op=ALU.mult),
                         [('ptl', slot), 'mlam'], [('ptl', slot)])

            def pv(n):
                G, il, r = blocks[n]
                gc0 = G * 512
                bo = 4 + (G % 2)
                cs, slot = info[n]
                firstg = (il == 0 and r == 0)
                lastg = (il == 4 * G + 3 and r == 3)
                P.op('pe', 'matmul', dict(out=PS(bo)[:, cs], lhsT=vh[k2][:, r, il, :], rhs=ptl[slot][:, cs], start=firstg, stop=lastg),
                     [('vh', k2), ('ptl', slot)], [('ps', bo)])
                if lastg:
                    g2 = G % 2
                    P.op('dve', 'reciprocal', dict(out=recm[g2][orow], in_=PS(bo)[drow, :]), [('ps', bo)], [('recm', g2)])
                    P.op('dve', 'tensor_tensor', dict(out=catT[orow, 5 + h // 2, gc0:gc0 + 512], in0=PS(bo)[orow, :], in1=recm[g2][orow], op=ALU.mult),
                         [('ps', bo), ('recm', g2)], [('catT3', h, G)])

            for n in range(NB + LA):
                if n < NB:
                    qk(n)
                if n - LA >= 0:
                    pv(n - LA)
        P.barrier()

        if stop == 'B3':
            raise _Stop()
        mem.top = base_top
        yacc = T('yacc', F32, NT, D)
        ln_sb = T('ln_sb', F32, 4, D)
        woutb = T('woutb', BF16, 8, D)
        cd_top = mem.top
        NB3 = 3
        xin = [T('xin%d' % k, F32, D) for k in range(2)]
        zt = [T('zt%d' % k, F32, D) for k in range(NB3)]
        zn = [T('zn%d' % k, F32, D) for k in range(NB3)]
        zb = [T('zb%d' % k, BF16, D) for k in range(2)]
        lst = [T('lst%d' % k, F32, 2, 6) for k in range(NB3)]
        lmv = [T('lmv%d' % k, F32, 2) for k in range(NB3)]
        lrs = [T('lrs%d' % k, F32, 1) for k in range(NB3)]
        for c in range(8):
            P.dma('pool', woutb[:, c, :], w_out[l, c * 128:(c + 1) * 128, :], [], ['woutb'], 'win')
        P.dma('sp', ln_sb, lnp[l].rearrange("a p n -> p a n"), [], ['ln_sb'], 'tabE')

        def ln_stats(t, zsrc, zres):
            k3 = t % NB3
            P.op('dve', 'bn_stats', dict(out=lst[k3][:, 0, :], in_=zsrc[:, 0:512]), [zres], [('lst', k3)])
            P.op('dve', 'bn_stats', dict(out=lst[k3][:, 1, :], in_=zsrc[:, 512:1024]), [zres], [('lst', k3)])
            P.op('dve', 'bn_aggr', dict(out=lmv[k3], in_=lst[k3].rearrange("p a b -> p (a b)")), [('lst', k3)], [('lmv', k3)])
            P.op('act', 'activation', dict(out=lrs[k3], in_=lmv[k3][:, 1:2], func=AF.Sqrt, bias=LN_EPS), [('lmv', k3)], [('lrs', k3)])

        def ln_norm(t, zsrc, zres, gi):
            k3 = t % NB3
            P.op('dve', 'reciprocal', dict(out=lrs[k3], in_=lrs[k3]), [('lrs', k3)], [('lrs', k3)])
            P.op('dve', 'tensor_scalar', dict(out=zn[k3], in0=zsrc, scalar1=lmv[k3][:, 0:1], scalar2=lrs[k3][:, 0:1], op0=ALU.subtract, op1=ALU.mult),
                 [zres, ('lmv', k3), ('lrs', k3)], [('zn', k3)])
            P.op('pool', 'tensor_tensor', dict(out=zn[k3], in0=zn[k3], in1=ln_sb[:, gi, :], op=ALU.mult), [('zn', k3), 'ln_sb'], [('zn', k3)])

        def c_mm(t):
            tc_ = slice(t * 128, (t + 1) * 128)
            k2 = t % 2
            k3 = t % NB3
            b0, b1 = 0 + 2 * k2, 1 + 2 * k2
            P.dma('sp', xin[k2], x_src[tc_, :], [], [('xin', k2)], ('xin', k2))
            for hf, bank in ((0, b0), (1, b1)):
                for c in range(8):
                    P.op('pe', 'matmul', dict(out=PS(bank), lhsT=catT[:, c, tc_], rhs=woutb[:, c, hf * 512:(hf + 1) * 512], start=(c == 0), stop=(c == 7)),
                         [('catT', t), ('catT2', t)] + [('catT3', h, t // 4) for h in range(6)] + ['woutb'], [('ps', bank)])
                P.op('dve', 'scalar_tensor_tensor', dict(out=zt[k3][:, hf * 512:(hf + 1) * 512], in0=xin[k2][:, hf * 512:(hf + 1) * 512],
                                                         scalar=ALPHA, in1=PS(bank), op0=ALU.mult, op1=ALU.add),
                     [('xin', k2), ('ps', bank)], [('zt', k3)])

        def c_fin(t):
            tc_ = slice(t * 128, (t + 1) * 128)
            k2 = t % 2
            k3 = t % NB3
            P.op('pool', 'tensor_tensor', dict(out=zt[k3], in0=zn[k3], in1=ln_sb[:, 1, :], op=ALU.add), [('zn', k3), 'ln_sb'], [('zt', k3)])
            P.op('act', 'copy', dict(out=zb[k2], in_=zt[k3]), [('zt', k3)], [('zb', k2)])
            P.op('act', 'mul', dict(out=yacc[:, t, :], in_=zt[k3], mul=ALPHA), [('zt', k3)], [('yacc', t)])
            transpose_to(zb[k2], ('zb', k2), 8, bufA[:, :, tc_], ('x1T', t), 4 + k2)

        skew([c_mm,
              lambda t: ln_stats(t, zt[t % NB3], ('zt', t % NB3)),
              lambda t: ln_norm(t, zt[t % NB3], ('zt', t % NB3), 0),
              c_fin], NT)
        P.barrier()

        if stop == 'C':
            raise _Stop()
        mem.top = OFFS['woutb']
        x1T = bufA
        EB = 4
        aT4 = V(OFFS['bufB'], BF16, EB, 2, TOK)
        wgu = [T('wgu%d' % k, BF16, 8, 512) for k in range(2)]
        wdn = T('wdn', BF16, EB, 2, D)
        lg = T('lg', F32, NT, 16); sc = T('sc', F32, NT, 16); bz = T('bz', F32, NT, 16)
        w1 = T('w1', F32, NT, 16); w2 = T('w2', F32, NT, 16); w3 = T('w3', F32, NT, 16)
        g1 = T('g1', F32, NT * 4); g2_ = T('g2', F32, NT * 4); gs = T('gs', F32, NT * 4); oh = T('oh', F32, NT * 4)
        gm = T('gm', F32, NT)
        gates = T('gates', BF16, NT, 16)
        gT = T('gT', BF16, TOK)
        sgl = [T('sgl%d' % k, F32, 512) for k in range(2)]
        tml = [T('tml%d' % k, F32, 512) for k in range(2)]
        BIG = 1.0e4
        for t in range(NT):
            for c in range(8):
                P.op('pe', 'matmul', dict(out=PS(0)[:, t * 16:(t + 1) * 16], lhsT=x1T[:, c, t * 128:(t + 1) * 128], rhs=wrt[:, c, :],
                                                       start=(c == 0), stop=(c == 7)), [('x1T', t), 'wrt'], [('ps', 0)])
        lgf = lg.rearrange("p a b -> p (a b)")
        scf = sc.rearrange("p a b -> p (a b)"); bzf = bz.rearrange("p a b -> p (a b)")
        w1f = w1.rearrange("p a b -> p (a b)"); w2f = w2.rearrange("p a b -> p (a b)"); w3f = w3.rearrange("p a b -> p (a b)")
        P.op('act', 'activation', dict(out=scf, in_=PS(0)[:, 0:256], func=AF.Sigmoid), [('ps', 0)], ['sc'])
        P.op('dve', 'tensor_tensor', dict(out=bzf, in0=scf, in1=rbias_sb, op=ALU.add), ['sc', 'rbias'], ['bz'])
        bz4 = bzf.rearrange("p (g x) -> p g x", x=4)
        w14 = w1f.rearrange("p (g x) -> p g x", x=4)
        w24 = w2f.rearrange("p (g x) -> p g x", x=4)
        R_ = ['bz', 'w1', 'w2', 'w3', 'g1', 'g2', 'gs', 'oh', 'gm', 'sc']
        def dv(name, kw):
            P.op('dve', name, kw, R_, R_)
        dv('tensor_reduce', dict(out=g1, in_=bz4, axis=AX.X, op=ALU.max))
        dv('tensor_tensor', dict(out=w14, in0=bz4, in1=g1.unsqueeze(2).broadcast_to([128, NT * 4, 4]), op=ALU.is_equal))
        dv('scalar_tensor_tensor', dict(out=w1f, in0=w1f, scalar=-BIG, in1=bzf, op0=ALU.mult, op1=ALU.add))
        dv('tensor_reduce', dict(out=g2_, in_=w14, axis=AX.X, op=ALU.max))
        dv('tensor_tensor', dict(out=gs, in0=g1, in1=g2_, op=ALU.add))
        gs3 = gs.rearrange("p (t g) -> p t g", g=4)
        oh3 = oh.rearrange("p (t g) -> p t g", g=4)
        dv('tensor_reduce', dict(out=gm, in_=gs3, axis=AX.X, op=ALU.max))
        dv('tensor_tensor', dict(out=oh3, in0=gs3, in1=gm.unsqueeze(2).broadcast_to([128, NT, 4]), op=ALU.is_equal))
        dv('tensor_scalar', dict(out=oh, in0=oh, scalar1=-1.0, scalar2=BIG, op0=ALU.add, op1=ALU.mult))
        dv('tensor_tensor', dict(out=w14, in0=bz4, in1=oh.unsqueeze(2).broadcast_to([128, NT * 4, 4]), op=ALU.add))
        dv('tensor_reduce', dict(out=gm, in_=w1, axis=AX.X, op=ALU.max))
        dv('tensor_tensor', dict(out=w2, in0=w1, in1=gm.unsqueeze(2).broadcast_to([128, NT, 16]), op=ALU.is_equal))
        dv('scalar_tensor_tensor', dict(out=w1f, in0=w2f, scalar=-BIG, in1=w1f, op0=ALU.mult, op1=ALU.add))
        dv('tensor_reduce', dict(out=gm, in_=w1, axis=AX.X, op=ALU.max))
        dv('tensor_tensor', dict(out=w3, in0=w1, in1=gm.unsqueeze(2).broadcast_to([128, NT, 16]), op=ALU.is_equal))
        dv('tensor_tensor', dict(out=w2f, in0=w2f, in1=w3f, op=ALU.add))
        dv('tensor_tensor', dict(out=w2f, in0=w2f, in1=scf, op=ALU.mult))
        dv('tensor_reduce', dict(out=gm, in_=w2, axis=AX.X, op=ALU.add))
        dv('reciprocal', dict(out=gm, in_=gm))
        P.op('dve', 'tensor_tensor', dict(out=gates, in0=w2, in1=gm.unsqueeze(2).broadcast_to([128, NT, 16]), op=ALU.mult), R_, ['gates'])
        pb = PSB(1)
        for t in range(8):
            P.op('pe', 'transpose', dict(out=pb[0:16, t * 128:(t + 1) * 128], in_=gates[:, t, :], identity=ident), ['gates', 'ident'], [('ps', 1)])
        P.op('act', 'copy', dict(out=gT[0:16, 0:1024], in_=pb[0:16, 0:1024]), [('ps', 1)], ['gT'])
        pb2 = PSB(2)
        for t in range(8, NT):
            P.op('pe', 'transpose', dict(out=pb2[0:16, (t - 8) * 128:(t - 7) * 128], in_=gates[:, t, :], identity=ident), ['gates', 'ident'], [('ps', 2)])
        P.op('act', 'copy', dict(out=gT[0:16, 1024:2048], in_=pb2[0:16, 0:1024]), [('ps', 2)], ['gT'])

        for eb in range(16 // EB):
            for ei in range(EB):
                ex_ = eb * EB + ei
                ws = wgu[ex_ % 2]
                for c in range(8):
                    P.dma('pool', ws[:, c, :], w_gu[l, ex_, c * 128:(c + 1) * 128, :], [], [('wgu', ex_ % 2)], ('wgu', ex_ % 2))
                P.dma('pool', wdn[:, ei], w_dn[l, ex_].rearrange("(c p) n -> p c n", p=128), [], [('wdn', ei)], ('wdn', ei))
                for G in range(4):
                    gc = slice(G * 512, (G + 1) * 512)
                    bg = 4 + (G % 2)
                    P.op('pe', 'matmul', dict(out=PS(bg), lhsT=sel_sb[0:16, ex_, :], rhs=gT[0:16, gc], start=True, stop=True),
                         ['sel', 'gT'], [('ps', bg)])
                    for fc in range(2):
                        s2 = (G * 2 + fc) % 2
                        bh, bu = 0 + 2 * s2, 1 + 2 * s2
                        for (bank, c0) in ((bh, fc * 128), (bu, 256 + fc * 128)):
                            for c in range(8):
                                P.op('pe', 'matmul', dict(out=PS(bank), lhsT=ws[:, c, c0:c0 + 128], rhs=x1T[:, c, gc],
                                                                                                  start=(c == 0), stop=(c == 7)),
                                     [('wgu', ex_ % 2), ('x1T', 4 * G), ('x1T', 4 * G + 1), ('x1T', 4 * G + 2), ('x1T', 4 * G + 3)], [('ps', bank)])
                        P.op('act', 'activation', dict(out=sgl[s2], in_=PS(bh), func=AF.Silu), [('ps', bh)], [('sgl', s2)])
                        P.op('dve', 'tensor_tensor', dict(out=tml[s2], in0=PS(bu), in1=sgl[s2], op=ALU.mult), [('ps', bu), ('sgl', s2)], [('tml', s2)])
                        P.op('dve', 'tensor_tensor', dict(out=aT4[:, ei, fc, gc], in0=tml[s2], in1=PS(bg), op=ALU.mult),
                             [('tml', s2), ('ps', bg)], [('aT', ei, G)])
            for t in range(NT):
                tc_ = slice(t * 128, (t + 1) * 128)
                for hf in range(2):
                    bank = 6 + hf
                    n = 0
                    for ei in range(EB):
                        for fc in range(2):
                            P.op('pe', 'matmul', dict(out=PS(bank), lhsT=aT4[:, ei, fc, tc_], rhs=wdn[:, ei, fc, hf * 512:(hf + 1) * 512],
                                                                                                  start=(n == 0), stop=(n == 2 * EB - 1)),
                                 [('aT', ei, t // 4), ('wdn', ei)], [('ps', bank)])
                            n += 1
                    P.op('dve', 'tensor_tensor', dict(out=yacc[:, t, hf * 512:(hf + 1) * 512], in0=yacc[:, t, hf * 512:(hf + 1) * 512],
                                                                                in1=PS(bank), op=ALU.add), [('yacc', t), ('ps', bank)], [('yacc', t)])
        P.barrier()
        def d_fin(t):
            tc_ = slice(t * 128, (t + 1) * 128)
            k2 = t % 2
            k3 = t % NB3
            P.op('pool', 'tensor_tensor', dict(out=zt[k3], in0=zn[k3], in1=ln_sb[:, 3, :], op=ALU.add), [('zn', k3), 'ln_sb'], [('zt', k3)])
            P.dma('sp', x_dst[tc_, :], zt[k3], [('zt', k3)], [], 'xout')
            if not last:
                P.op('act', 'copy', dict(out=zb[k2], in_=zt[k3]), [('zt', k3)], [('zb', k2)])
                transpose_to(zb[k2], ('zb', k2), 8, bufA[:, :, tc_], ('xT', t), 4 + k2)

        skew([lambda t: ln_stats(t, yacc[:, t, :], ('yacc', t)),
              lambda t: ln_norm(t, yacc[:, t, :], ('yacc', t), 2),
              d_fin], NT)
        P.barrier()

    except _Stop:
        P.barrier()
    P.emit()
    es.close()
    return nc


def _tables(j):
    bf = ml_dtypes.bfloat16
    tb = {}
    i_ = np.arange(NT)[:, None]
    t_ = np.arange(128)[None, :]
    pos = ((4 * i_ + j) * 128 + t_).astype(np.float64)
    inv64 = 10000.0 ** (-np.arange(0, 64, 2, dtype=np.float64) / 64)
    ang = pos[:, :, None] * inv64[None, None, :]
    tb["t_rcos"] = np.cos(ang).transpose(1, 0, 2).reshape(128, NT * 32).astype(np.float32)
    tb["t_rsin"] = np.sin(ang).transpose(1, 0, 2).reshape(128, NT * 32).astype(np.float32)
    inv32 = 10000.0 ** (-np.arange(0, 32, 2, dtype=np.float64) / 32)
    a2 = pos.reshape(-1)[None, :] * inv32[:, None]
    sc = 96.0 ** -0.5
    cq = np.ones((96, TOK)) * sc
    sq = np.zeros((96, TOK))
    cq[64:80] = np.cos(a2) * sc; cq[80:96] = np.cos(a2) * sc
    sq[64:80] = np.sin(a2) * sc; sq[80:96] = np.sin(a2) * sc
    tb["t_cosq"] = cq.astype(np.float32); tb["t_sinq"] = sq.astype(np.float32)
    tb["t_cosk"] = np.concatenate([np.cos(a2), np.cos(a2)], 0).astype(np.float32)
    tb["t_sink"] = np.concatenate([np.sin(a2), np.sin(a2)], 0).astype(np.float32)
    gam = 1.0 - 2.0 ** (-5.0 - np.arange(4, dtype=np.float64))
    jj = np.arange(128)[:, None]; ii = np.arange(128)[None, :]
    dec = np.zeros((128, 4, 128))
    for h in range(4):
        dec[:, h, :] = np.where(ii >= jj, gam[h] ** np.maximum(ii - jj, 0), 0.0) / 8.0
    tb["t_decay"] = dec.reshape(128, 512).astype(np.float32)
    p = np.arange(128)
    xit = np.zeros((128, 2, 128))
    for c in range(2):
        hh = 2 * c + p // 64
        xit[:, c, :] = gam[hh][:, None] ** (np.arange(128)[None, :] + 1.0)
    tb["t_xit"] = xit.reshape(128, 256).astype(np.float32)
    zeta = np.zeros((128, 4, 64))
    for h in range(4):
        zeta[:, h, :] = (gam[h] ** (127.0 - np.arange(128)))[:, None] / 8.0
    tb["t_zeta"] = zeta.reshape(128, 256).astype(np.float32)
    bd = np.zeros((128, 2, 128))
    for c in range(2):
        bd[:, c, :] = (p[:, None] // 64 == np.arange(128)[None, :] // 64)
    tb["t_bd"] = bd.reshape(128, 256).astype(np.float32)
    ct = np.zeros((128, 2, 5)); cs = np.zeros((128, 2, 5))
    for c in range(2):
        Dd = gam[2 * c + p // 64] ** 128.0
        ct[:, c, 0] = Dd ** 4
        for r in range(4):
            ct[:, c, 1 + r] = Dd ** (3 - r)
        cs[:, c, 0] = Dd ** j
        for r in range(4):
            cs[:, c, 1 + r] = Dd ** (j - 1 - r) if r < j else 0.0
    eye = np.eye(128)
    tb["t_diagt"] = (ct[:, :, :, None] * eye[:, None, None, :]).reshape(128, 1280).astype(np.float32)
    tb["t_diags"] = (cs[:, :, :, None] * eye[:, None, None, :]).reshape(128, 1280).astype(np.float32)
    kl = np.arange(128)[:, None]; ql = np.arange(128)[None, :]
    caus = (kl <= ql).astype(np.float32); upper = (kl > ql).astype(np.float32)
    sm = np.zeros((128, 5, 3, 128), np.float32)
    for c in range(4):
        if c == j:
            sm[:, c] = caus[:, None, :]
        elif c == j - 1:
            sm[:, c] = upper[:, None, :]
    if j == 0:
        sm[:, 4] = upper[:, None, :]
    tb["t_swam"] = sm.reshape(128, 5 * 384).astype(bf)
    mm = np.zeros((128, 4, 128), np.float32)
    for r in range(4):
        if r < j:
            mm[:, r] = 1.0
        elif r == j:
            mm[:, r] = caus
    tb["t_mlam"] = mm.reshape(128, 512).astype(bf)
    tb["t_ident"] = np.eye(128, dtype=np.float32).astype(bf)
    sel = np.zeros((16, 16, 128), np.float32)
    for e in range(16):
        sel[e, e, :] = 1.0
    tb["t_sel"] = sel.reshape(16, 16 * 128).astype(bf)
    return tb


_TAB = {}
_NC = {}


def _shared(inp, ls):
    f = np.float32
    d = {}
    d["w_in"] = np.ascontiguousarray(inp["w_in"][ls], f)
    d["w_out"] = np.ascontiguousarray(inp["w_out"][ls], f)
    d["w_uq"] = np.ascontiguousarray(inp["mla_w_uq"][ls], f)
    d["w_ukv"] = np.ascontiguousarray(inp["mla_w_ukv"][ls], f)
    d["w_gu"] = np.ascontiguousarray(inp["exp_w_gate_up"][ls], f)
    d["w_dn"] = np.ascontiguousarray(inp["exp_w_down"][ls], f)
    d["w_rt"] = np.ascontiguousarray(inp["router_w"], f)
    n = len(ls)
    lnp = np.stack([np.stack([inp["ln1_g"][l], inp["ln1_b"][l], inp["ln2_g"][l], inp["ln2_b"][l]], 0) for l in ls], 0)
    d["lnp"] = np.ascontiguousarray(np.broadcast_to(lnp[:, :, None, :], (n, 4, 128, D)), f)
    d["gnw"] = np.ascontiguousarray(np.broadcast_to(np.asarray(inp["ret_gn_w"])[ls][:, None, :], (n, 128, 256)), f)
    d["qnw"] = np.ascontiguousarray(np.asarray(inp["mla_q_norm_w"])[ls].reshape(n, 3, 128).transpose(0, 2, 1), f)
    d["kvnw"] = np.ascontiguousarray(np.asarray(inp["mla_kv_norm_w"])[ls].reshape(n, 2, 128).transpose(0, 2, 1), f)
    d["sinks"] = np.ascontiguousarray(np.asarray(inp["swa_sinks"])[ls].reshape(n, 1, 6), f)
    rb = np.tile(np.asarray(inp["router_bias"], f)[None, :], (NT, 1)).reshape(1, 256)
    d["rbias"] = np.ascontiguousarray(np.broadcast_to(rb, (128, 256)), f)
    return d


def _run(xs, inp, ls, final, stop=None):
    key = (len(ls), final, stop)
    if key not in _NC:
        _NC[key] = build(len(ls), final_to_out=True, stop=stop)
    nc = _NC[key]
    sh = _shared(inp, ls)
    maps = []
    for c in range(8):
        j = c % 4
        if j not in _TAB:
            _TAB[j] = _tables(j)
        m = dict(sh)
        m.update(_TAB[j])
        m["x"] = xs[c]
        maps.append(m)
    res = run_bass_kernel_spmd(nc, maps, core_ids=list(range(8)))
    return [np.asarray(r["y"], np.float32) for r in res.results]


FUSED = True


def kernel(**inputs):
    inp = {k: np.asarray(v) for k, v in inputs.items()}
    x = np.asarray(inp["x"], np.float32)
    xs = []
    for c in range(8):
        b, j = c // 4, c % 4
        xt = x[b].reshape(64, 128, D)[j::4]
        xs.append(np.ascontiguousarray(xt.reshape(TOK, D)))
    if FUSED:
        ys = _run(xs, inp, [0, 1], True)
    else:
        ys = _run(xs, inp, [0], True)
        ys = _run(ys, inp, [1], True)
    out = np.zeros((2, 64, 128, D), np.float32)
    for c in range(8):
        b, j = c // 4, c % 4
        out[b, j::4] = ys[c].reshape(NT, 128, D)
    return out.reshape(2, 8192, D)
```

```python
import numpy as np
import ml_dtypes
import contextlib
import concourse.bass as bass
import concourse.mybir as mybir
from concourse.bass_utils import run_bass_kernel_spmd

F32 = mybir.dt.float32
BF16 = mybir.dt.bfloat16
U8 = mybir.dt.uint8
AF = mybir.ActivationFunctionType
ALU = mybir.AluOpType
AX = mybir.AxisListType

NT = 16
TOK = 2048
D = 1024
INC = 2336
ALPHA = 4.0 ** 0.25
LN_EPS = 1e-5
RMS_EPS = 1e-6
ENGS = ['pe', 'act', 'dve', 'pool', 'sp']
import os
KCUT = int(os.environ.get('KCUT', '0'))

EX_SK, EX_KN, EX_KP, EX_SV, EX_VM, EX_ROWS = 0, 128, 512, 544, 928, 1312
EXU_ROWS = 512


class Prog:
    def __init__(self, nc):
        self.nc = nc
        self.streams = {e: [] for e in ENGS}
        self.cnt = {e: 0 for e in ENGS}
        self.waited = {e: {} for e in ENGS}
        self.res = {}
        self.dcnt = {}

    def _deps(self, eng, reads, writes, dma_key=None):
        toks = []
        for r in reads:
            st = self.res.get(r)
            if st is not None and st[0] is not None:
                toks.append(st[0])
        for w in writes:
            st = self.res.get(w)
            if st is not None:
                if st[0] is not None and not (dma_key is not None and st[0][0] == 'd' and st[0][1] == dma_key):
                    toks.append(st[0])
                toks.extend(st[1])
        waits = []
        for t in toks:
            kind, p, c = t
            if kind == 'e' and p == eng and eng == 'pe':
                continue
            if kind == 'd':
                c = self.dcnt[p]
                t = (kind, p, c)
            k = (kind, p)
            if self.waited[eng].get(k, 0) >= c:
                continue
            self.waited[eng][k] = c
            waits.append(t)
        return waits

    def _upd(self, tok, reads, writes):
        for r in reads:
            if r in writes:
                continue
            st = self.res.setdefault(r, [None, []])
            st[1] = [x for x in st[1] if not (x[0] == tok[0] and x[1] == tok[1])] + [tok]
        for w in writes:
            self.res[w] = [tok, []]

    def op(self, eng, name, kw, reads=(), writes=()):
        fn = (lambda e, name=name, kw=kw: getattr(e, name)(**kw))
        waits = self._deps(eng, reads, writes)
        if eng == 'pe':
            src = kw.get('lhsT', kw.get('in_'))
            rg = src.base_partition() if callable(getattr(src, 'base_partition', None)) else 0
            if rg != getattr(self, '_last_rg', 0) and self.cnt['pe'] > 0:
                if self.waited['pe'].get(('e', 'pe'), 0) < self.cnt['pe']:
                    self.waited['pe'][('e', 'pe')] = self.cnt['pe']
                    waits.append(('e', 'pe', self.cnt['pe']))
            self._last_rg = rg
        self.cnt[eng] += 1
        tok = ('e', eng, self.cnt[eng])
        self.streams[eng].append((waits, fn, ('e', eng)))
        self._upd(tok, reads, writes)
        return tok

    def dma(self, q, out, in_, reads, writes, key):
        waits = self._deps(q, reads, writes, dma_key=key)
        self.dcnt[key] = self.dcnt.get(key, 0) + 16
        tok = ('d', key, self.dcnt[key])
        self.streams[q].append((waits, (lambda e, o=out, i=in_: e.dma_start(out=o, in_=i)), ('d', key)))
        self._upd(tok, reads, writes)
        return tok

    def custom(self, q, name, kw, reads, writes, key, incval):
        if os.environ.get('KNOCC'):
            return None
        fn = (lambda e, name=name, kw=kw: getattr(e, name)(**kw))
        waits = self._deps(q, reads, writes)
        self.dcnt[key] = self.dcnt.get(key, 0) + incval
        tok = ('d', key, self.dcnt[key])
        self.streams[q].append((waits, fn, ('c', key, incval)))
        self._upd(tok, reads, writes)
        return tok

    def barrier(self):
        toks = [('e', e, self.cnt[e]) for e in ENGS if self.cnt[e] > 0]
        toks += [('d', k, v) for k, v in self.dcnt.items() if not (isinstance(k, tuple) and k[0] == 'cc')]
        for eng in ENGS:
            waits = []
            for t in toks:
                k = (t[0], t[1])
                if self.waited[eng].get(k, 0) >= t[2]:
                    continue
                self.waited[eng][k] = t[2]
                waits.append(t)
            if waits:
                self.streams[eng].append((waits, None, None))
        self.res = {k: v for k, v in self.res.items() if isinstance(k, tuple) and k[0] == 'gx'}

    def emit(self):
        nc = self.nc
        with contextlib.ExitStack() as es:
            esem = {e: es.enter_context(nc.semaphore("s_" + e)) for e in ENGS}
            dsem = {k: es.enter_context(nc.semaphore("d_%d" % i)) for i, k in enumerate(self.dcnt)}
            block = es.enter_context(nc.Block())

            def run(name, h):
                for waits, fn, inc in self.streams[name]:
                    for kind, p, c in waits:
                        h.wait_ge(esem[p] if kind == 'e' else dsem[p], c)
                    if fn is None:
                        continue
                    ins = fn(h)
                    if inc[0] == 'e':
                        ins.then_inc(esem[inc[1]], 1)
                    elif inc[0] == 'd':
                        ins.then_inc(dsem[inc[1]], 16)
                    else:
                        ins.then_inc(dsem[inc[1]], inc[2])

            @block.tensor
            def _(h):
                run('pe', h)

            @block.scalar
            def _(h):
                run('act', h)

            @block.vector
            def _(h):
                run('dve', h)

            @block.gpsimd
            def _(h):
                run('pool', h)

            @block.sync
            def _(h):
                run('sp', h)


def build(nl, final_to_out=True, stop=None):
    nc = bass.Bass("TRN2", target_bir_lowering=False)
    P = Prog(nc)

    class _Stop(Exception):
        pass

    def din(name, shape, dt=F32):
        return nc.dram_tensor(name, list(shape), dt, kind="ExternalInput").ap()

    x_in = din("x", [TOK, D])
    w_in = din("w_in", [nl, D, INC])
    w_out = din("w_out", [nl, D, D])
    w_uq = din("w_uq", [nl, 384, 576])
    w_ukv = din("w_ukv", [nl, 256, 768])
    w_gu = din("w_gu", [nl, 16, D, 512])
    w_dn = din("w_dn", [nl, 16, 256, D])
    w_rt = din("w_rt", [D, 16])
    lnp = din("lnp", [nl, 4, 128, D])
    gnw = din("gnw", [nl, 128, 256])
    qnw = din("qnw", [nl, 128, 3])
    kvnw = din("kvnw", [nl, 128, 2])
    sinks = din("sinks", [nl, 1, 6])
    rbias = din("rbias", [128, 256])
    t_rcos = din("t_rcos", [128, NT * 32]); t_rsin = din("t_rsin", [128, NT * 32])
    t_cosq = din("t_cosq", [96, TOK]); t_sinq = din("t_sinq", [96, TOK])
    t_cosk = din("t_cosk", [32, TOK]); t_sink = din("t_sink", [32, TOK])
    t_decay = din("t_decay", [128, 512]); t_xit = din("t_xit", [128, 256]); t_zeta = din("t_zeta", [128, 256])
    t_bd = din("t_bd", [128, 256]); t_diagt = din("t_diagt", [128, 1280]); t_diags = din("t_diags", [128, 1280])
    t_swam = din("t_swam", [128, 5 * 384], BF16); t_mlam = din("t_mlam", [128, 512], BF16)
    t_ident = din("t_ident", [128, 128], BF16)
    t_sel = din("t_sel", [16, 16 * 128], BF16)
    y_out = nc.dram_tensor("y", [TOK, D], F32, kind="ExternalOutput").ap()

    CH = [160, 256, 128, 192, 192, 192, 192]
    ex = [[nc.dram_tensor("ex%d_%d" % (l, i), [r_, 2048], BF16).ap() for i, r_ in enumerate(CH)] for l in range(nl)]
    gx = [[nc.dram_tensor("gx%d_%d" % (l, i), [4 * r_, 2048], BF16).ap() for i, r_ in enumerate(CH)] for l in range(nl)]
    exu = [[nc.dram_tensor("exu%d_%d" % (l, i), [256, 1024], F32).ap() for i in range(2)] for l in range(nl)]
    gxu = [[nc.dram_tensor("gxu%d_%d" % (l, i), [1024, 1024], F32).ap() for i in range(2)] for l in range(nl)]
    xres = [nc.dram_tensor("xres%d" % l, [TOK, D], F32).ap() for l in range(nl)]

    es = contextlib.ExitStack()
    ARENA = 204 * 1024
    arena = es.enter_context(nc.sbuf_tensor("arena", [128, ARENA], U8))
    psb = [es.enter_context(nc.psum_tensor("ps%d" % k, [128, 512], F32)) for k in range(8)]

    def PS(k):
        return psb[k][:]

    def PSB(k):
        return psb[k][:].bitcast(BF16)

    class Mem:
        def __init__(self):
            self.top = 0

        def alloc(self, nbytes):
            off = self.top
            self.top = off + ((nbytes + 63) // 64) * 64
            assert self.top <= ARENA, ("SBUF overflow", self.top)
            return off

    mem = Mem()

    def V(off, dt, *dims):
        esz = 4 if dt == F32 else 2
        n = 1
        for d_ in dims:
            n *= d_
        ap = arena[:, off:off + n * esz].bitcast(dt)
        if len(dims) == 2:
            ap = ap.rearrange("p (a b) -> p a b", a=dims[0])
        elif len(dims) == 3:
            ap = ap.rearrange("p (a b c) -> p a b c", a=dims[0], b=dims[1])
        elif len(dims) == 4:
            ap = ap.rearrange("p (a b c d) -> p a b c d", a=dims[0], b=dims[1], c=dims[2])
        return ap

    OFFS = {}

    def T(nm, dt, *dims):
        esz = 4 if dt == F32 else 2
        n = 1
        for d_ in dims:
            n *= d_
        OFFS[nm] = mem.alloc(n * esz)
        return V(OFFS[nm], dt, *dims)

    ident = T('ident', BF16, 128)
    ones_bf = T('ones', BF16, 128)
    bufA = T('bufA', BF16, 8, TOK)
    bufB = T('bufB', BF16, 8, TOK)
    rbias_sb = T('rbias', F32, 256)
    wrt = T('wrt', BF16, 8, 16)
    sel_sb = T('sel', BF16, 16, 128)
    P.dma('sp', ident, t_ident, [], ['ident'], 'c0')
    P.dma('sp', rbias_sb, rbias, [], ['rbias'], 'c0')
    P.dma('sp', sel_sb[0:16], t_sel.rearrange("p (a b) -> p a b", a=16), [], ['sel'], 'c0')
    P.dma('pool', wrt, w_rt.rearrange("(c p) n -> p c n", p=128), [], ['wrt'], 'c1')
    P.op('dve', 'memset', dict(ap=ones_bf, constant=1.0), [], ['ones'])
    base_top = mem.top

    def transpose_to(src_bf, src_res, n, dst, dst_res, bank):
        pb = PSB(bank)
        for c in range(n):
            P.op('pe', 'transpose', dict(out=pb[:, c * 128:(c + 1) * 128], in_=src_bf[:, c * 128:(c + 1) * 128], identity=ident),
                 [src_res, 'ident'], [('ps', bank)])
        P.op('act', 'copy', dict(out=dst, in_=pb[:, 0:n * 128].rearrange("p (a b) -> p a b", a=n)),
             [('ps', bank)], [dst_res])

    def skew(stages, n):
        ns = len(stages)
        for step in range(n + ns - 1):
            for si in range(ns - 1, -1, -1):
                t = step - si
                if 0 <= t < n:
                    stages[si](t)


    xs = [T('xs%d' % k, F32, D) for k in range(2)]
    xb = [T('xb%d' % k, BF16, D) for k in range(2)]
    for t in range(NT):
        k = t % 2
        P.dma('sp', xs[k], x_in[t * 128:(t + 1) * 128, :], [], [('xs', k)], ('xs', k))
        P.op('dve', 'tensor_copy', dict(out=xb[k], in_=xs[k]), [('xs', k)], [('xb', k)])
        transpose_to(xb[k], ('xb', k), 8, bufA[:, :, t * 128:(t + 1) * 128], ('xT', t), t % 2)
    P.barrier()
    mem.top = base_top

    try:
      if stop == 'init':
          raise _Stop()
      for l in range(nl):
        last = (l == nl - 1)
        x_src = x_in if l == 0 else xres[l - 1]
        x_dst = y_out if (last and final_to_out) else xres[l]
        xT = bufA
        catT = bufB
        mem.top = base_top
        sqT = T('sqT', BF16, 3, TOK)
        cqnT = T('cqnT', BF16, 3, TOK)
        pers2_top = mem.top
        qT = T('qT', BF16, 2, TOK); qxiT = T('qxiT', BF16, 2, TOK); kT = T('kT', BF16, 2, TOK)
        v_tm = T('v_tm', BF16, NT, 256); sg_tm = T('sg_tm', BF16, NT, 256)
        pers_top = mem.top
        win = T('win', BF16, 8, 1184)
        wukv = T('wukv', BF16, 2, 768)
        wkrp = T('wkrp', BF16, 8, 32)
        kvg = T('kvg', F32, 2)
        rcos = T('rcos', F32, NT, 32); rsin = T('rsin', F32, NT, 32)
        _save = mem.top
        mem.top = OFFS['bufB']
        cosk = T('cosk', F32, TOK); sink_t = T('sink_t', F32, TOK)
        sv_st = T('sv_st', BF16, NT, 2, 192)
        assert mem.top <= OFFS['bufB'] + 8 * TOK * 2
        mem.top = _save
        decay = T('decay', F32, 512); xit = T('xit', F32, 2, 128); zeta = T('zeta', F32, 256); bd = T('bd', F32, 256)
        vm_st = [T('vm_st%d' % k, BF16, 384) for k in range(2)]
        sk_st = T('sk_st', BF16, TOK)
        kp_st = T('kp_st', BF16, TOK)
        ckvn = [T('ckvn%d' % k, BF16, 2, 512) for k in range(2)]
        xit_bf = T('xit_bf', BF16, 2, 128); zeta_bf = T('zeta_bf', BF16, 256)
        sq_sc = [T('sq%d' % k, BF16, 512) for k in range(3)]
        rstd = T('rstd', F32, 512)
        kn_st = [T('kn%d' % k, BF16, 512) for k in range(4)]
        rt = [T('rt%d' % k, F32, 256) for k in range(4)]
        qk_tm = [T('qk_tm%d' % k, BF16, 512) for k in range(2)]
        kz = [T('kz%d' % k, BF16, 256) for k in range(2)]
        u_st = [T('u_st%d' % k, F32, 256) for k in range(2)]
        kpt = [T('kpt%d' % k, F32, 512) for k in range(2)]

        for c in range(8):
            P.dma('pool', win[:, c, 0:1024], w_in[l, c * 128:(c + 1) * 128, 0:1024], [], ['win'], 'win')
            P.dma('pool', win[:, c, 1024:1152], w_in[l, c * 128:(c + 1) * 128, 1536:1664], [], ['win'], 'win')
        P.dma('pool', wukv, w_ukv[l].rearrange("(c p) n -> p c n", p=128), [], ['wukv'], 'win')
        P.dma('sp', kvg, kvnw[l], [], ['kvg'], 'tabA')
        for dst, src in ((rcos, t_rcos), (rsin, t_rsin)):
            P.dma('sp', dst, src.rearrange("p (a b) -> p a b", a=NT), [], ['tabA_'], 'tabA')
        P.dma('sp', cosk[0:32], t_cosk, [], ['tabA_'], 'tabA')
        P.dma('sp', sink_t[0:32], t_sink, [], ['tabA_'], 'tabA')
        P.dma('sp', decay, t_decay, [], ['tabA_'], 'tabA')
        P.dma('sp', xit, t_xit.rearrange("p (a b) -> p a b", a=2), [], ['tabA_'], 'tabA')
        P.dma('sp', zeta, t_zeta, [], ['tabA_'], 'tabA')
        P.dma('sp', bd, t_bd, [], ['tabA_'], 'tabA')
        P.op('dve', 'memset', dict(ap=sv_st, constant=1.0), [], ['sv_st'])
        P.op('dve', 'tensor_copy', dict(out=xit_bf, in_=xit), ['tabA_'], ['xit_bf'])
        P.op('dve', 'tensor_copy', dict(out=zeta_bf, in_=zeta), ['tabA_'], ['zeta_bf'])
        for kc in range(2):
            P.op('dve', 'tensor_scalar', dict(out=wukv[:, kc, :], in0=wukv[:, kc, :], scalar1=kvg[:, kc:kc + 1],
                                                         scalar2=None, op0=ALU.mult), ['wukv', 'kvg'], ['wukv'])

        if stop == 'A0':
            raise _Stop()
        def tm0(t):
            tc_ = slice(t * 128, (t + 1) * 128)
            k2 = t % 2
            ba, bb, bc = 0 + k2, 2 + k2, 4
            for (bank, c0, n) in ((ba, 0, 512), (bb, 512, 512), (bc, 1024, 128)):
                for c in range(8):
                    P.op('pe', 'matmul', dict(out=PS(bank)[:, 0:n], lhsT=xT[:, c, tc_], rhs=win[:, c, c0:c0 + n], start=(c == 0), stop=(c == 7)),
                         [('xT', t), 'win'], [('ps', bank)])

        def tm1(t):
            k2 = t % 2
            ba, bb, bc = 0 + k2, 2 + k2, 4
            pa = PS(ba).rearrange("p (h d) -> p h d", h=8)
            x1, x2 = pa[:, :, 0:32], pa[:, :, 32:64]
            cb = rcos[:, t, :].unsqueeze(1).broadcast_to([128, 8, 32])
            sb_ = rsin[:, t, :].unsqueeze(1).broadcast_to([128, 8, 32])
            r0, r1, r2, r3 = [rt[i].rearrange("p (h d) -> p h d", h=8) for i in range(4)]
            P.op('dve', 'tensor_tensor', dict(out=r0, in0=x1, in1=cb, op=ALU.mult), [('ps', ba), 'tabA_'], ['rt0'])
            P.op('dve', 'tensor_tensor', dict(out=r1, in0=x2, in1=sb_, op=ALU.mult), [('ps', ba)], ['rt1'])
            P.op('dve', 'tensor_tensor', dict(out=r2, in0=x1, in1=sb_, op=ALU.mult), [('ps', ba)], ['rt2'])
            P.op('dve', 'tensor_tensor', dict(out=r3, in0=x2, in1=cb, op=ALU.mult), [('ps', ba)], ['rt3'])
            P.op('act', 'copy', dict(out=v_tm[:, t, :], in_=PS(bb)[:, 0:256]), [('ps', bb)], [('v_tm', t)])
            P.op('act', 'activation', dict(out=sg_tm[:, t, :], in_=PS(bb)[:, 256:512], func=AF.Silu), [('ps', bb)], [('sg_tm', t)])
            P.op('act', 'copy', dict(out=sv_st[:, t, :, 64:128], in_=PS(bc)[:, 0:128].rearrange("p (a b) -> p a b", a=2)), [('ps', bc)], ['sv_st'])

        def tm2(t):
            k2 = t % 2
            r0, r1, r2, r3 = [rt[i].rearrange("p (h d) -> p h d", h=8) for i in range(4)]
            qk3 = qk_tm[k2].rearrange("p (h d) -> p h d", h=8)
            P.op('pool', 'tensor_tensor', dict(out=qk3[:, :, 0:32], in0=r0, in1=r1, op=ALU.subtract), ['rt0', 'rt1'], [('qk_tm', k2)])
            P.op('pool', 'tensor_tensor', dict(out=qk3[:, :, 32:64], in0=r2, in1=r3, op=ALU.add), ['rt2', 'rt3', ('qk_tm', k2)], [('qk_tm', k2)])

        def tm3(t):
            tc_ = slice(t * 128, (t + 1) * 128)
            k2 = t % 2
            bt = 6 + k2
            pb = PSB(bt)
            for c in range(4):
                P.op('pe', 'transpose', dict(out=pb[:, c * 128:(c + 1) * 128], in_=qk_tm[k2][:, c * 128:(c + 1) * 128], identity=ident),
                     [('qk_tm', k2), 'ident'], [('ps', bt)])
            pb3 = pb[:, 0:512].rearrange("p (a b) -> p a b", a=4)
            P.op('act', 'copy', dict(out=qT[:, :, tc_], in_=pb3[:, 0:2, :]), [('ps', bt)], [('qT', t)])
            P.op('act', 'copy', dict(out=kT[:, :, tc_], in_=pb3[:, 2:4, :]), [('ps', bt)], [('kT', t)])
            P.op('pool', 'tensor_tensor', dict(out=kz[k2], in0=qk_tm[k2][:, 256:512], in1=zeta_bf, op=ALU.mult), [('qk_tm', k2), 'zeta_bf'], [('kz', k2)])

        def tm4(t):
            tc_ = slice(t * 128, (t + 1) * 128)
            k2 = t % 2
            bu = 5
            P.op('dve', 'tensor_tensor', dict(out=qxiT[:, :, tc_], in0=qT[:, :, tc_], in1=xit_bf, op=ALU.mult), [('qT', t), 'xit_bf'], [('qxiT', t)])
            for c in range(2):
                P.op('pe', 'matmul', dict(out=PS(bu)[:, c * 128:128 + c * 128], lhsT=kz[k2][:, c * 128:(c + 1) * 128],
                                          rhs=v_tm[:, t, c * 128:(c + 1) * 128], start=True, stop=True),
                     [('kz', k2), ('v_tm', t)], [('ps', bu)])
            P.op('dve', 'tensor_tensor', dict(out=u_st[k2], in0=PS(bu)[:, 0:256], in1=bd, op=ALU.mult), [('ps', bu), 'tabA_'], [('u_st', k2)])
            P.dma('sp', exu[l][t // 8][(t % 8) * 32:(t % 8 + 1) * 32, :].rearrange("a b -> (a b)").rearrange("(p x) -> p x", p=128), u_st[k2],
                  [('u_st', k2)], [('exw', 'u', t // 8)], ('exw', 'u', t // 8))

        skew([tm0, tm1, tm2, tm3, tm4], NT)

        if stop == 'A1':
            raise _Stop()
        for c in range(8):
            P.dma('pool', win[:, c, 0:512], w_in[l, c * 128:(c + 1) * 128, 1024:1536], [], ['win'], 'win')
            P.dma('pool', win[:, c, 512:1184], w_in[l, c * 128:(c + 1) * 128, 1664:2336], [], ['win'], 'win')
        P.op('dve', 'tensor_scalar', dict(out=wkrp[:, :, 0:16], in0=win[:, :, 1168:1184], scalar1=-1.0, scalar2=None,
                                          op0=ALU.mult), ['win'], ['wkrp'])
        P.op('dve', 'tensor_copy', dict(out=wkrp[:, :, 16:32], in_=win[:, :, 1152:1168]), ['win', 'wkrp'], ['wkrp'])
        rg = [[0, 1, 2, 3], [4, 5, 6, 7]]
        for hf in range(2):
            P.dma('sp', ex[l][3 + hf].rearrange("a b -> (a b)").rearrange("(t p x) -> p t x", t=8, p=128),
                  sv_st[:, hf * 8:(hf + 1) * 8].rearrange("p t k x -> p t (k x)"), ['sv_st'], [('exw', 3 + hf)], ('exw', 3 + hf))
        for ci in range(2):
            P.custom('pool', 'collective_compute', dict(kind="AllGather", op=ALU.bypass, replica_groups=rg, ins=[exu[l][ci].opt()], outs=[gxu[l][ci].opt()]),
                 [('exw', 'u', ci)], [('gx', 'u', ci)], ('cc', l, 'u', ci), 1)
        for ci in (3, 4):
            P.custom('pool', 'collective_compute', dict(kind="AllGather", op=ALU.bypass, replica_groups=rg, ins=[ex[l][ci].opt()], outs=[gx[l][ci].opt()]),
                 [('exw', ci)], [('gx', ci)], ('cc', l, ci), 1)
        def fmm(G, bank, c0, m, extra_w=None):
            gc = slice(G * 512, (G + 1) * 512)
            for c in range(8):
                wsrc = (win[:, c, c0:c0 + m] if extra_w is None else extra_w[:, c, :])
                P.op('pe', 'matmul', dict(out=PS(bank)[0:m, :], lhsT=wsrc, rhs=xT[:, c, gc], start=(c == 0), stop=(c == 7)),
                     ['win', 'wkrp'], [('ps', bank)])

        def kv0(G):
            gc = slice(G * 512, (G + 1) * 512)
            cb = 6 if G % 2 == 0 else 4
            for c2 in range(2):
                fmm(G, cb + c2, 896 + c2 * 128, 128)
                P.op('act', 'activation', dict(out=sq_sc[c2], in_=PS(cb + c2), func=AF.Square), [('ps', cb + c2)], [('sq', c2)])
            fmm(G, 0, 384, 128)
            P.op('act', 'copy', dict(out=sk_st[:, gc], in_=PS(0)), [('ps', 0)], ['sk_st'])
            fmm(G, 1, 1152, 32)
            fmm(G, 2, 0, 32, extra_w=wkrp)
            kp0, kp1 = kpt[0], kpt[1]
            P.op('dve', 'tensor_tensor', dict(out=kp0[0:32], in0=PS(1)[0:32], in1=cosk[0:32, gc], op=ALU.mult), [('ps', 1), 'tabA_'], ['kp0'])
            P.op('dve', 'tensor_tensor', dict(out=kp1[0:32], in0=PS(2)[0:32], in1=sink_t[0:32, gc], op=ALU.mult), [('ps', 2), 'tabA_'], ['kp1'])
            P.op('dve', 'tensor_tensor', dict(out=kp_st[0:32, gc], in0=kp0[0:32], in1=kp1[0:32], op=ALU.add), ['kp0', 'kp1'], ['kp_st'])

        def kv1(G):
            cb = 6 if G % 2 == 0 else 4
            ck = ckvn[G % 2]
            for c2 in range(2):
                P.op('pe', 'matmul', dict(out=PS(3), lhsT=ones_bf, rhs=sq_sc[c2], start=(c2 == 0), stop=(c2 == 1)), [('sq', c2), 'ones'], [('ps', 3)])
            P.op('act', 'activation', dict(out=rstd, in_=PS(3), func=AF.Sqrt, scale=1.0 / 256.0, bias=RMS_EPS), [('ps', 3)], ['rstd'])
            P.op('dve', 'reciprocal', dict(out=rstd, in_=rstd), ['rstd'], ['rstd'])
            for c2 in range(2):
                P.op('dve', 'tensor_tensor', dict(out=ck[:, c2, :], in0=PS(cb + c2), in1=rstd, op=ALU.mult), [('ps', cb + c2), 'rstd'], [('ckvn', G % 2)])

        def kv2(G):
            gc = slice(G * 512, (G + 1) * 512)
            cb = 6 if G % 2 == 0 else 4
            ck = ckvn[G % 2]
            nb = 0
            for h in range(6):
                bank = cb + (nb % 2)
                nb += 1
                ks = kn_st[h % 4]
                for kc in range(2):
                    P.op('pe', 'matmul', dict(out=PS(bank)[0:64, :], lhsT=wukv[:, kc, h * 128:h * 128 + 64], rhs=ck[:, kc, :], start=(kc == 0), stop=(kc == 1)),
                         [('ckvn', G % 2), 'wukv'], [('ps', bank)])
                P.op('act', 'copy', dict(out=ks[0:64], in_=PS(bank)[0:64, :]), [('ps', bank)], [('kn', h % 4)])
                P.dma('sp', ex[l][1 + h // 4][(h % 4) * 64:(h % 4 + 1) * 64, gc], ks[0:64], [('kn', h % 4)], [('exw', 1 + h // 4)], ('exw', 1 + h // 4))
            vv = wukv.rearrange("p c (h x) -> p c h x", h=6)
            for tt in range(4):
                t = G * 4 + tt
                bank = cb + (nb % 2)
                nb += 1
                for kc in range(2):
                    P.op('pe', 'matmul', dict(out=PS(bank)[:, 0:384].rearrange("p (h x) -> p h x", h=6), lhsT=ck[:, kc, tt * 128:(tt + 1) * 128],
                                              rhs=vv[:, kc, :, 64:128], start=(kc == 0), stop=(kc == 1)), [('ckvn', G % 2), 'wukv'], [('ps', bank)])
                P.op('act', 'copy', dict(out=vm_st[tt % 2], in_=PS(bank)[:, 0:384]), [('ps', bank)], [('vm_st', tt % 2)])
                P.dma('sp', ex[l][5 + t // 8][(t % 8) * 24:(t % 8 + 1) * 24, :].rearrange("a b -> (a b)").rearrange("(p x) -> p x", p=128), vm_st[tt % 2],
                      [('vm_st', tt % 2)], [('exw', 5 + t // 8)], ('exw', 5 + t // 8))

        skew([kv0, kv1, kv2], 4)
        P.dma('sp', ex[l][0][0:128, :], sk_st, ['sk_st'], [('exw', 0)], ('exw', 0))
        P.dma('sp', ex[l][0][128:160, :], kp_st[0:32], ['kp_st'], [('exw', 0)], ('exw', 0))
        for ci in (0, 1, 2, 5, 6):
            P.custom('pool', 'collective_compute', dict(kind="AllGather", op=ALU.bypass, replica_groups=rg, ins=[ex[l][ci].opt()], outs=[gx[l][ci].opt()]),
                 [('exw', ci)], [('gx', ci)], ('cc', l, ci), 1)
        def q0(G):
            gc = slice(G * 512, (G + 1) * 512)
            cb = 2 if G % 2 == 0 else 5
            for c3 in range(3):
                fmm(G, cb + c3, 512 + c3 * 128, 128)
                P.op('act', 'activation', dict(out=sq_sc[c3], in_=PS(cb + c3), func=AF.Square), [('ps', cb + c3)], [('sq', c3)])
            for c3 in range(3):
                fmm(G, 0, c3 * 128, 128)
                P.op('act', 'mul', dict(out=sqT[:, c3, gc], in_=PS(0), mul=0.125), [('ps', 0)], [('sqT', G)])

        def q1(G):
            gc = slice(G * 512, (G + 1) * 512)
            cb = 2 if G % 2 == 0 else 5
            for c3 in range(3):
                P.op('pe', 'matmul', dict(out=PS(1), lhsT=ones_bf, rhs=sq_sc[c3], start=(c3 == 0), stop=(c3 == 2)), [('sq', c3), 'ones'], [('ps', 1)])
            P.op('act', 'activation', dict(out=rstd, in_=PS(1), func=AF.Sqrt, scale=1.0 / 384.0, bias=RMS_EPS), [('ps', 1)], ['rstd'])
            P.op('dve', 'reciprocal', dict(out=rstd, in_=rstd), ['rstd'], ['rstd'])
            for c3 in range(3):
                P.op('dve', 'tensor_tensor', dict(out=cqnT[:, c3, gc], in0=PS(cb + c3), in1=rstd, op=ALU.mult), [('ps', cb + c3), 'rstd'], [('cqnT', G)])

        skew([q0, q1], 4)
        P.barrier()
        if stop == 'A' or stop == 'X':
            raise _Stop()
        gxl = gx[l]

        if stop == 'X':
            raise _Stop()
        mem.top = pers_top
        diagt = T('diagt', F32, 2, 5, 128); diags = T('diags', F32, 2, 5, 128)
        gn_sb = T('gn_sb', F32, 256)
        decay = T('decay', F32, 512)
        Tst = T('Tst', F32, 2, 128)
        Sbd = T('Sbd', BF16, NT, 2, 128)
        ug = [T('ug%d' % k, F32, 4, 2, 128) for k in range(2)]
        sacc = T('sacc', F32, 2, 128)
        sTm = [T('sTm%d' % k, BF16, 512) for k in range(2)]
        bst = [T('bst%d' % k, F32, 4, 6) for k in range(2)]; mv = [T('mv%d' % k, F32, 4, 2) for k in range(2)]; rs4 = [T('rs4%d' % k, F32, 4) for k in range(2)]
        yb = [T('yb%d' % k, F32, 256) for k in range(2)]
        yb2 = [T('yb2%d' % k, F32, 256) for k in range(2)]
        ro = [T('ro%d' % k, BF16, 256) for k in range(2)]
        P.dma('sp', diagt, t_diagt.rearrange("p (a b c) -> p a b c", a=2, b=5), [], ['tabB'], 'tabB')
        P.dma('sp', diags, t_diags.rearrange("p (a b c) -> p a b c", a=2, b=5), [], ['tabB'], 'tabB')
        P.dma('sp', gn_sb, gnw[l], [], ['tabB'], 'tabB')
        P.dma('sp', decay, t_decay, [], ['tabB'], 'tabB')
        P.op('dve', 'memset', dict(ap=Tst, constant=0.0), [], ['Tst'])
        def b1_rec(i):
            k2 = i % 2
            for r in range(4):
                P.dma('sp', ug[k2][:, r].rearrange("p c x -> p (c x)"),
                      gxu[l][i // 8][r * 256 + (i % 8) * 32:r * 256 + (i % 8 + 1) * 32, :].rearrange("a b -> (a b)").rearrange("(p x) -> p x", p=128),
                      [('gx', 'u', i // 8)], [('ug', k2)], ('ug', k2))
            for (dg, bank) in ((diags, 6), (diagt, 7)):
                for c in range(2):
                    for sidx in range(5):
                        rhs = Tst[:, c, :] if sidx == 0 else ug[k2][:, sidx - 1, c, :]
                        P.op('pe', 'matmul', dict(out=PS(bank)[:, c * 128:(c + 1) * 128], lhsT=dg[:, c, sidx, :], rhs=rhs, start=(sidx == 0), stop=(sidx == 4)),
                             ['Tst', ('ug', k2), 'tabB'], [('ps', bank)])
            P.op('act', 'copy', dict(out=Sbd[:, i].rearrange("p c x -> p (c x)"), in_=PS(6)[:, 0:256]), [('ps', 6)], [('Sbd', i)])
            P.op('act', 'copy', dict(out=Tst.rearrange("p c x -> p (c x)"), in_=PS(7)[:, 0:256]), [('ps', 7)], ['Tst'])

        def b1_s0(t):
            tc_ = slice(t * 128, (t + 1) * 128)
            k2 = t % 2
            bs = 0 + k2
            b1_rec(t)
            for h in range(4):
                rows = slice((h % 2) * 64, (h % 2) * 64 + 64)
                P.op('pe', 'matmul', dict(out=PS(bs)[:, h * 128:(h + 1) * 128], lhsT=kT[rows, h // 2, tc_], rhs=qT[rows, h // 2, tc_], start=True, stop=True),
                     [('kT', t), ('qT', t)], [('ps', bs)])
            P.op('dve', 'tensor_tensor', dict(out=sTm[k2], in0=PS(bs), in1=decay, op=ALU.mult), [('ps', bs), 'tabB'], [('sTm', k2)])

        def b1_s1(t):
            tc_ = slice(t * 128, (t + 1) * 128)
            k2 = t % 2
            bo = 2 + k2
            for c in range(2):
                P.op('pe', 'matmul', dict(out=PS(bo)[:, c * 128:(c + 1) * 128], lhsT=qxiT[:, c, tc_], rhs=Sbd[:, t, c, :], start=True, stop=False),
                     [('qxiT', t), ('Sbd', t)], [('ps', bo)])
                for hh in range(2):
                    h = 2 * c + hh
                    P.op('pe', 'matmul', dict(out=PS(bo)[:, h * 64:(h + 1) * 64], lhsT=sTm[k2][:, h * 128:(h + 1) * 128],
                                              rhs=v_tm[:, t, h * 64:(h + 1) * 64], start=False, stop=(hh == 1)),
                         [('sTm', k2), ('v_tm', t)], [('ps', bo)])

        def b1_s2(t):
            k2 = t % 2
            bo = 2 + k2
            for h in range(4):
                P.op('dve', 'bn_stats', dict(out=bst[k2][:, h, :], in_=PS(bo)[:, h * 64:(h + 1) * 64]), [('ps', bo)], [('bst', k2)])
            for h in range(4):
                P.op('dve', 'bn_aggr', dict(out=mv[k2][:, h, :], in_=bst[k2][:, h, :]), [('bst', k2)], [('mv', k2)])
            P.op('act', 'activation', dict(out=rs4[k2], in_=mv[k2][:, :, 1], func=AF.Sqrt, bias=LN_EPS), [('mv', k2)], [('rs4', k2)])

        def b1_s3(t):
            k2 = t % 2
            bo = 2 + k2
            P.op('dve', 'reciprocal', dict(out=rs4[k2], in_=rs4[k2]), [('rs4', k2)], [('rs4', k2)])
            for h in range(4):
                P.op('dve', 'tensor_scalar', dict(out=yb[k2][:, h * 64:(h + 1) * 64], in0=PS(bo)[:, h * 64:(h + 1) * 64],
                                                  scalar1=mv[k2][:, h, 0:1], scalar2=rs4[k2][:, h:h + 1], op0=ALU.subtract, op1=ALU.mult),
                     [('ps', bo), ('mv', k2), ('rs4', k2)], [('yb', k2)])
            P.op('dve', 'tensor_tensor', dict(out=yb2[k2], in0=yb[k2], in1=gn_sb, op=ALU.mult), [('yb', k2), 'tabB'], [('yb2', k2)])
            P.op('act', 'copy', dict(out=yb[k2], in_=sg_tm[:, t, :]), [('sg_tm', t), ('yb', k2), ('yb2', k2)], [('yb', k2)])
            P.op('dve', 'tensor_tensor', dict(out=ro[k2], in0=yb2[k2], in1=yb[k2], op=ALU.mult), [('yb2', k2), ('yb', k2)], [('ro', k2)])

        def b1_s4(t):
            tc_ = slice(t * 128, (t + 1) * 128)
            k2 = t % 2
            transpose_to(ro[k2], ('ro', k2), 2, catT[:, 0:2, tc_], ('catT', t), 4 + k2)

        skew([b1_s0, b1_s1, b1_s2, b1_s3, b1_s4], NT)
        P.barrier()

        if stop == 'B1':
            raise _Stop()
        mem.top = pers2_top
        skt = T('skt', BF16, 2, 4, TOK)
        svg = T('svg', BF16, 4, NT, 384)
        swam = T('swam', BF16, 5, 384)
        esr = T('esr', BF16, 6, 128)
        esf = T('esf', F32, 8)
        onesv = T('onesv', BF16, 2, 128)
        pt = [T('pt%d' % k, BF16, 384) for k in range(4)]
        ptm = [T('ptm%d' % k, BF16, 384) for k in range(4)]
        rec = [T('rec%d' % k, F32, 384) for k in range(2)]
        for r in range(4):
            base = r * 160
            P.dma('sp', skt[:, 0, r, :], gxl[0][base:base + 128, :], [('gx', 0)], ['skt'], 'tabC')
            P.dma('sp', skt[0:64, 1, r, :], gxl[0][base + 64:base + 128, :], [('gx', 0)], ['skt'], 'tabC')
            P.dma('sp', skt[64:128, 1, r, :], gxl[0][base:base + 64, :], [('gx', 0)], ['skt'], 'tabC')
            for hf in range(2):
                P.dma('sp', svg[:, r, hf * 8:(hf + 1) * 8], gxl[3 + hf][r * 192:(r + 1) * 192, :].rearrange("a b -> (a b)").rearrange("(t p x) -> p t x", t=8, p=128),
                      [('gx', 3 + hf)], ['svg'], 'tabC')
        P.dma('sp', swam, t_swam.rearrange("p (a b) -> p a b", a=5), [], ['swam'], 'tabC')
        P.dma('sp', esf[0:1, 0:6], sinks[l], [], ['esf'], 'tabC')
        P.op('act', 'activation', dict(out=esf[0:1, 0:6], in_=esf[0:1, 0:6], func=AF.Exp), ['esf'], ['esf'])
        P.op('dve', 'tensor_copy', dict(out=esr[0:1], in_=esf[0:1, 0:6].unsqueeze(2).broadcast_to([1, 6, 128])), ['esf'], ['esr'])
        P.op('dve', 'memset', dict(ap=onesv[0:1], constant=0.0), [], ['onesv'])
        P.op('dve', 'memset', dict(ap=onesv[0:1, 0, 64:128], constant=1.0), ['onesv'], ['onesv'])
        P.op('dve', 'memset', dict(ap=onesv[0:1, 1, 0:64], constant=1.0), ['onesv'], ['onesv'])
        svg5 = svg.rearrange("p r t (k x) -> p r t k x", k=2)
        LA2 = 2
        nu = 0
        for t in range(NT):
            tc_ = slice(t * 128, (t + 1) * 128)
            cands = [(0, t), (1, t), (2, t), (3, t)] + ([(3, t - 1)] if t > 0 else [])
            bo = [4 + (t % 2) * 2, 5 + (t % 2) * 2]
            units = [(ci, par) for ci in range(len(cands)) for par in range(2)]
            NU = len(units)
            uinfo = {}

            def qk2(u):
                nonlocal nu
                ci, par = units[u]
                r, il = cands[ci]
                midx = ci if ci < 4 else 4
                kc_ = slice(il * 128, (il + 1) * 128)
                slot = nu % 4
                bank = slot
                nu += 1
                uinfo[u] = slot
                o = par * 64
                for g in range(3):
                    hq = 2 * g + par
                    hk = hq // 3
                    var = 0 if (o == 0) == (hk == 0) else 1
                    P.op('pe', 'matmul', dict(out=PS(bank)[:, g * 128:(g + 1) * 128], lhsT=skt[o:o + 64, var, r, kc_], rhs=sqT[o:o + 64, hq // 2, tc_],
                                              start=True, stop=True), ['skt', ('sqT', t // 4)], [('ps', bank)])
                P.op('act', 'activation', dict(out=pt[slot], in_=PS(bank)[:, 0:384], func=AF.Exp), [('ps', bank)], [('pt', slot)])
                P.op('pool', 'tensor_tensor', dict(out=ptm[slot], in0=pt[slot], in1=swam[:, midx, :], op=ALU.mult),
                     [('pt', slot), 'swam'], [('ptm', slot)])

            def pv2(u):
                ci, par = units[u]
                r, il = cands[ci]
                slot = uinfo[u]
                for g in range(3):
                    hq = 2 * g + par
                    hk = hq // 3
                    lw = svg5[:, r, il, hk, 64:192] if par == 0 else svg5[:, r, il, hk, 0:128]
                    P.op('pe', 'matmul', dict(out=PS(bo[par])[:, g * 128:(g + 1) * 128], lhsT=lw, rhs=ptm[slot][:, g * 128:(g + 1) * 128],
                                              start=(ci == 0 and g == 0), stop=False), ['svg', ('ptm', slot)], [('ps', bo[par])])

            for u in range(NU + LA2):
                if u < NU:
                    qk2(u)
                if u - LA2 >= 0:
                    pv2(u - LA2)
            for hq in range(6):
                par = hq % 2
                col = (hq // 2) * 128
                P.op('pe', 'matmul', dict(out=PS(bo[par])[:, col:col + 128], lhsT=onesv[0:1, par, :], rhs=esr[0:1, hq, :], start=False, stop=(hq >= 4)),
                     ['onesv', 'esr'], [('ps', bo[par])])
            for par in range(2):
                orow = slice(par * 64, par * 64 + 64)
                drow = slice((1 - par) * 64, (1 - par) * 64 + 64)
                P.op('dve', 'reciprocal', dict(out=rec[par][orow], in_=PS(bo[par])[drow, 0:384]), [('ps', bo[par])], [('rec', par)])
                P.op('dve', 'tensor_tensor', dict(out=catT[orow, 2:5, tc_], in0=PS(bo[par])[orow, 0:384].rearrange("p (a b) -> p a b", a=3),
                                                  in1=rec[par][orow].rearrange("p (a b) -> p a b", a=3), op=ALU.mult),
                     [('ps', bo[par]), ('rec', par)], [('catT2', t)])
        P.barrier()

        if stop == 'B2':
            raise _Stop()
        mem.top = pers2_top
        wuq = T('wuq', BF16, 3, 576); wuqp = T('wuqp', BF16, 3, 576)
        qg = T('qg', F32, 3)
        cosq = T('cosq', F32, TOK); sinq = T('sinq', F32, TOK)
        mlam = T('mlam', BF16, 4, 128)
        _save = mem.top
        mem.top = OFFS['bufA']
        kth = [T('kth%d' % k, BF16, 4, TOK) for k in range(2)]
        assert mem.top <= OFFS['bufA'] + 8 * TOK * 2
        mem.top = _save
        vh = [T('vh%d' % k, BF16, 4, NT, 128) for k in range(2)]
        qTh = [T('qTh%d' % k, BF16, TOK) for k in range(2)]
        q1 = [T('q1%d' % k, F32, 512) for k in range(2)]
        q2 = [T('q2%d' % k, F32, 512) for k in range(2)]
        NPT = 6
        ptl = [T('ptl%d' % k, BF16, 512) for k in range(NPT)]
        recm = [T('recm%d' % k, F32, 512) for k in range(2)]
        P.dma('pool', wuq, w_uq[l].rearrange("(c p) n -> p c n", p=128), [], ['wuq'], 'win')
        P.dma('sp', qg, qnw[l], [], ['qg'], 'tabD')
        P.dma('sp', cosq[0:96], t_cosq, [], ['tabD_'], 'tabD')
        P.dma('sp', sinq[0:96], t_sinq, [], ['tabD_'], 'tabD')
        P.dma('sp', mlam, t_mlam.rearrange("p (a b) -> p a b", a=4), [], ['mlam'], 'tabD')
        for kc in range(3):
            P.op('dve', 'tensor_scalar', dict(out=wuq[:, kc, :], in0=wuq[:, kc, :], scalar1=qg[:, kc:kc + 1], scalar2=None, op0=ALU.mult),
                 ['wuq', 'qg'], ['wuq'])
        P.op('dve', 'memset', dict(ap=wuqp, constant=0.0), [], ['wuqp'])
        w4 = wuq.rearrange("p c (h x) -> p c h x", h=6)
        wp4 = wuqp.rearrange("p c (h x) -> p c h x", h=6)
        for kc in range(3):
            P.op('dve', 'tensor_scalar', dict(out=wp4[:, kc, :, 64:80], in0=w4[:, kc, :, 80:96], scalar1=-1.0, scalar2=None, op0=ALU.mult),
                 ['wuq', 'wuqp'], ['wuqp'])
            P.op('dve', 'tensor_copy', dict(out=wp4[:, kc, :, 80:96], in_=w4[:, kc, :, 64:80]), ['wuq', 'wuqp'], ['wuqp'])
        P.op('dve', 'memset', dict(ap=vh[0][:, :, :, 64:128], constant=1.0), [], [('vh', 0)])
        P.op('dve', 'memset', dict(ap=vh[1][:, :, :, 0:64], constant=1.0), [], [('vh', 1)])
        for k in range(2):
            for r in range(4):
                P.dma('sp', kth[k][64:96, r, :], gxl[0][r * 160 + 128:r * 160 + 160, :], [('gx', 0)], [('kth', k)], ('kth', k))
        def mla_loads(h):
            k2 = h % 2
            for r in range(4):
                rows_ = CH[1 + h // 4]
                kb = r * rows_ + (h % 4) * 64
                P.dma('sp', kth[k2][0:64, r, :], gxl[1 + h // 4][kb:kb + 64, :], [('gx', 1 + h // 4)], [('kth', k2)], ('kth', k2))
                vcols = slice(0, 64) if k2 == 0 else slice(64, 128)
                for half in range(2):
                    vsrc = gxl[5 + half][r * 192:(r + 1) * 192, :].rearrange("a b -> (a b)").rearrange("(t p x) -> p t x", t=8, p=128)[:, :, h * 64:(h + 1) * 64]
                    P.dma('sp', vh[k2][:, r, half * 8:(half + 1) * 8, vcols], vsrc, [('gx', 5 + half)], [('vh', k2)], ('vh', k2))

        def mla_q(h):
            k2 = h % 2
            for G in range(4):
                gc = slice(G * 512, (G + 1) * 512)
                b1, b2 = 6, 7
                for kc in range(3):
                    P.op('pe', 'matmul', dict(out=PS(b1)[0:96, :], lhsT=wuq[:, kc, h * 96:(h + 1) * 96], rhs=cqnT[:, kc, gc],
                                              start=(kc == 0), stop=(kc == 2)), ['wuq', ('cqnT', G)], [('ps', b1)])
                for kc in range(3):
                    P.op('pe', 'matmul', dict(out=PS(b2)[0:96, :], lhsT=wuqp[:, kc, h * 96:(h + 1) * 96], rhs=cqnT[:, kc, gc],
                                              start=(kc == 0), stop=(kc == 2)), ['wuqp', ('cqnT', G)], [('ps', b2)])
                g2 = G % 2
                P.op('dve', 'tensor_tensor', dict(out=q1[g2][0:96], in0=PS(b1)[0:96, :], in1=cosq[0:96, gc], op=ALU.mult),
                     [('ps', b1), 'tabD_'], [('q1', g2)])
                P.op('dve', 'tensor_tensor', dict(out=q2[g2][0:96], in0=PS(b2)[0:96, :], in1=sinq[0:96, gc], op=ALU.mult),
                     [('ps', b2), 'tabD_'], [('q2', g2)])
                P.op('dve', 'tensor_tensor', dict(out=qTh[k2][0:96, gc], in0=q1[g2][0:96], in1=q2[g2][0:96], op=ALU.add),
                     [('q1', g2), ('q2', g2)], [('qTh', k2, G)])

        LA = 3
        mla_loads(0)
        mla_q(0)
        nkt = 0
        for h in range(6):
            k2 = h % 2
            if h + 1 < 6:
                mla_loads(h + 1)
                mla_q(h + 1)
            par = h % 2
            orow = slice(par * 64, par * 64 + 64)
            drow = slice((1 - par) * 64, (1 - par) * 64 + 64)
            blocks = [(G, il, r) for G in range(4) for il in range(4 * G + 4) for r in range(4)]
            NB = len(blocks)
            info = {}

            def qk(n):
                nonlocal nkt
                G, il, r = blocks[n]
                gc0 = G * 512
                a = max(0, il - 4 * G)
                qs = slice(gc0 + a * 128, gc0 + 512)
                cs = slice(a * 128, 512)
                bank = nkt % 4
                slot = nkt % NPT
                nkt += 1
                info[n] = (cs, slot)
                P.op('pe', 'matmul', dict(out=PS(bank)[:, cs], lhsT=kth[k2][0:96, r, il * 128:(il + 1) * 128], rhs=qTh[k2][0:96, qs],
                                          start=True, stop=True), [('kth', k2), ('qTh', k2, G)], [('ps', bank)])
                P.op('act', 'activation', dict(out=ptl[slot][:, cs], in_=PS(bank)[:, cs], func=AF.Exp), [('ps', bank)], [('ptl', slot)])
                if il >= 4 * G:
                    ms = slice(a * 128, a * 128 + 128)
                    P.op('dve', 'tensor_tensor', dict(out=ptl[slot][:, ms], in0=ptl[slot][:, ms], in1=mlam[:, r, :], op=ALU.mult),
                         [('ptl', slot), 'mlam'], [('ptl', slot)])

            def pv(n):
                G, il, r = blocks[n]
                gc0 = G * 512
                bo = 4 + (G % 2)
                cs, slot = info[n]
                firstg = (il == 0 and r == 0)
                lastg = (il == 4 * G + 3 and r == 3)
                P.op('pe', 'matmul', dict(out=PS(bo)[:, cs], lhsT=vh[k2][:, r, il, :], rhs=ptl[slot][:, cs], start=firstg, stop=lastg),
                     [('vh', k2), ('ptl', slot)], [('ps', bo)])
                if lastg:
                    g2 = G % 2
                    P.op('dve', 'reciprocal', dict(out=recm[g2][orow], in_=PS(bo)[drow, :]), [('ps', bo)], [('recm', g2)])
                    P.op('dve', 'tensor_tensor', dict(out=catT[orow, 5 + h // 2, gc0:gc0 + 512], in0=PS(bo)[orow, :], in1=recm[g2][orow], op=ALU.mult),
                         [('ps', bo), ('recm', g2)], [('catT3', h, G)])

            for n in range(NB + LA):
                if n < NB:
                    qk(n)
                if n - LA >= 0:
                    pv(n - LA)
        P.barrier()

        if stop == 'B3':
            raise _Stop()
        mem.top = base_top
        yacc = T('yacc', F32, NT, D)
        ln_sb = T('ln_sb', F32, 4, D)
        woutb = T('woutb', BF16, 8, D)
        cd_top = mem.top
        NB3 = 3
        xin = [T('xin%d' % k, F32, D) for k in range(2)]
        zt = [T('zt%d' % k, F32, D) for k in range(NB3)]
        zn = [T('zn%d' % k, F32, D) for k in range(NB3)]
        zb = [T('zb%d' % k, BF16, D) for k in range(2)]
        lst = [T('lst%d' % k, F32, 2, 6) for k in range(NB3)]
        lmv = [T('lmv%d' % k, F32, 2) for k in range(NB3)]
        lrs = [T('lrs%d' % k, F32, 1) for k in range(NB3)]
        for c in range(8):
            P.dma('pool', woutb[:, c, :], w_out[l, c * 128:(c + 1) * 128, :], [], ['woutb'], 'win')
        P.dma('sp', ln_sb, lnp[l].rearrange("a p n -> p a n"), [], ['ln_sb'], 'tabE')

        def ln_stats(t, zsrc, zres):
            k3 = t % NB3
            P.op('dve', 'bn_stats', dict(out=lst[k3][:, 0, :], in_=zsrc[:, 0:512]), [zres], [('lst', k3)])
            P.op('dve', 'bn_stats', dict(out=lst[k3][:, 1, :], in_=zsrc[:, 512:1024]), [zres], [('lst', k3)])
            P.op('dve', 'bn_aggr', dict(out=lmv[k3], in_=lst[k3].rearrange("p a b -> p (a b)")), [('lst', k3)], [('lmv', k3)])
            P.op('act', 'activation', dict(out=lrs[k3], in_=lmv[k3][:, 1:2], func=AF.Sqrt, bias=LN_EPS), [('lmv', k3)], [('lrs', k3)])

        def ln_norm(t, zsrc, zres, gi):
            k3 = t % NB3
            P.op('dve', 'reciprocal', dict(out=lrs[k3], in_=lrs[k3]), [('lrs', k3)], [('lrs', k3)])
            P.op('dve', 'tensor_scalar', dict(out=zn[k3], in0=zsrc, scalar1=lmv[k3][:, 0:1], scalar2=lrs[k3][:, 0:1], op0=ALU.subtract, op1=ALU.mult),
                 [zres, ('lmv', k3), ('lrs', k3)], [('zn', k3)])
            P.op('pool', 'tensor_tensor', dict(out=zn[k3], in0=zn[k3], in1=ln_sb[:, gi, :], op=ALU.mult), [('zn', k3), 'ln_sb'], [('zn', k3)])

        def c_mm(t):
            tc_ = slice(t * 128, (t + 1) * 128)
            k2 = t % 2
            k3 = t % NB3
            b0, b1 = 0 + 2 * k2, 1 + 2 * k2
            P.dma('sp', xin[k2], x_src[tc_, :], [], [('xin', k2)], ('xin', k2))
            for hf, bank in ((0, b0), (1, b1)):
                for c in range(8):
                    P.op('pe', 'matmul', dict(out=PS(bank), lhsT=catT[:, c, tc_], rhs=woutb[:, c, hf * 512:(hf + 1) * 512], start=(c == 0), stop=(c == 7)),
                         [('catT', t), ('catT2', t)] + [('catT3', h, t // 4) for h in range(6)] + ['woutb'], [('ps', bank)])
                P.op('dve', 'scalar_tensor_tensor', dict(out=zt[k3][:, hf * 512:(hf + 1) * 512], in0=xin[k2][:, hf * 512:(hf + 1) * 512],
                                                         scalar=ALPHA, in1=PS(bank), op0=ALU.mult, op1=ALU.add),
                     [('xin', k2), ('ps', bank)], [('zt', k3)])

        def c_fin(t):
            tc_ = slice(t * 128, (t + 1) * 128)
            k2 = t % 2
            k3 = t % NB3
            P.op('pool', 'tensor_tensor', dict(out=zt[k3], in0=zn[k3], in1=ln_sb[:, 1, :], op=ALU.add), [('zn', k3), 'ln_sb'], [('zt', k3)])
            P.op('act', 'copy', dict(out=zb[k2], in_=zt[k3]), [('zt', k3)], [('zb', k2)])
            P.op('act', 'mul', dict(out=yacc[:, t, :], in_=zt[k3], mul=ALPHA), [('zt', k3)], [('yacc', t)])
            transpose_to(zb[k2], ('zb', k2), 8, bufA[:, :, tc_], ('x1T', t), 4 + k2)

        skew([c_mm,
              lambda t: ln_stats(t, zt[t % NB3], ('zt', t % NB3)),
              lambda t: ln_norm(t, zt[t % NB3], ('zt', t % NB3), 0),
              c_fin], NT)
        P.barrier()

        if stop == 'C':
            raise _Stop()
        mem.top = OFFS['woutb']
        x1T = bufA
        EB = 4
        aT4 = V(OFFS['bufB'], BF16, EB, 2, TOK)
        wgu = [T('wgu%d' % k, BF16, 8, 512) for k in range(2)]
        wdn = T('wdn', BF16, EB, 2, D)
        lg = T('lg', F32, NT, 16); sc = T('sc', F32, NT, 16); bz = T('bz', F32, NT, 16)
        w1 = T('w1', F32, NT, 16); w2 = T('w2', F32, NT, 16); w3 = T('w3', F32, NT, 16)
        g1 = T('g1', F32, NT * 4); g2_ = T('g2', F32, NT * 4); gs = T('gs', F32, NT * 4); oh = T('oh', F32, NT * 4)
        gm = T('gm', F32, NT)
        gates = T('gates', BF16, NT, 16)
        gT = T('gT', BF16, TOK)
        sgl = [T('sgl%d' % k, F32, 512) for k in range(2)]
        tml = [T('tml%d' % k, F32, 512) for k in range(2)]
        BIG = 1.0e4
        for t in range(NT):
            for c in range(8):
                P.op('pe', 'matmul', dict(out=PS(0)[:, t * 16:(t + 1) * 16], lhsT=x1T[:, c, t * 128:(t + 1) * 128], rhs=wrt[:, c, :],
                                                       start=(c == 0), stop=(c == 7)), [('x1T', t), 'wrt'], [('ps', 0)])
        lgf = lg.rearrange("p a b -> p (a b)")
        scf = sc.rearrange("p a b -> p (a b)"); bzf = bz.rearrange("p a b -> p (a b)")
        w1f = w1.rearrange("p a b -> p (a b)"); w2f = w2.rearrange("p a b -> p (a b)"); w3f = w3.rearrange("p a b -> p (a b)")
        P.op('act', 'activation', dict(out=scf, in_=PS(0)[:, 0:256], func=AF.Sigmoid), [('ps', 0)], ['sc'])
        P.op('dve', 'tensor_tensor', dict(out=bzf, in0=scf, in1=rbias_sb, op=ALU.add), ['sc', 'rbias'], ['bz'])
        bz4 = bzf.rearrange("p (g x) -> p g x", x=4)
        w14 = w1f.rearrange("p (g x) -> p g x", x=4)
        w24 = w2f.rearrange("p (g x) -> p g x", x=4)
        R_ = ['bz', 'w1', 'w2', 'w3', 'g1', 'g2', 'gs', 'oh', 'gm', 'sc']
        def dv(name, kw):
            P.op('dve', name, kw, R_, R_)
        dv('tensor_reduce', dict(out=g1, in_=bz4, axis=AX.X, op=ALU.max))
        dv('tensor_tensor', dict(out=w14, in0=bz4, in1=g1.unsqueeze(2).broadcast_to([128, NT * 4, 4]), op=ALU.is_equal))
        dv('scalar_tensor_tensor', dict(out=w1f, in0=w1f, scalar=-BIG, in1=bzf, op0=ALU.mult, op1=ALU.add))
        dv('tensor_reduce', dict(out=g2_, in_=w14, axis=AX.X, op=ALU.max))
        dv('tensor_tensor', dict(out=gs, in0=g1, in1=g2_, op=ALU.add))
        gs3 = gs.rearrange("p (t g) -> p t g", g=4)
        oh3 = oh.rearrange("p (t g) -> p t g", g=4)
        dv('tensor_reduce', dict(out=gm, in_=gs3, axis=AX.X, op=ALU.max))
        dv('tensor_tensor', dict(out=oh3, in0=gs3, in1=gm.unsqueeze(2).broadcast_to([128, NT, 4]), op=ALU.is_equal))
        dv('tensor_scalar', dict(out=oh, in0=oh, scalar1=-1.0, scalar2=BIG, op0=ALU.add, op1=ALU.mult))
        dv('tensor_tensor', dict(out=w14, in0=bz4, in1=oh.unsqueeze(2).broadcast_to([128, NT * 4, 4]), op=ALU.add))
        dv('tensor_reduce', dict(out=gm, in_=w1, axis=AX.X, op=ALU.max))
        dv('tensor_tensor', dict(out=w2, in0=w1, in1=gm.unsqueeze(2).broadcast_to([128, NT, 16]), op=ALU.is_equal))
        dv('scalar_tensor_tensor', dict(out=w1f, in0=w2f, scalar=-BIG, in1=w1f, op0=ALU.mult, op1=ALU.add))
        dv('tensor_reduce', dict(out=gm, in_=w1, axis=AX.X, op=ALU.max))
        dv('tensor_tensor', dict(out=w3, in0=w1, in1=gm.unsqueeze(2).broadcast_to([128, NT, 16]), op=ALU.is_equal))
        dv('tensor_tensor', dict(out=w2f, in0=w2f, in1=w3f, op=ALU.add))
        dv('tensor_tensor', dict(out=w2f, in0=w2f, in1=scf, op=ALU.mult))
        dv('tensor_reduce', dict(out=gm, in_=w2, axis=AX.X, op=ALU.add))
        dv('reciprocal', dict(out=gm, in_=gm))
        P.op('dve', 'tensor_tensor', dict(out=gates, in0=w2, in1=gm.unsqueeze(2).broadcast_to([128, NT, 16]), op=ALU.mult), R_, ['gates'])
        pb = PSB(1)
        for t in range(8):
            P.op('pe', 'transpose', dict(out=pb[0:16, t * 128:(t + 1) * 128], in_=gates[:, t, :], identity=ident), ['gates', 'ident'], [('ps', 1)])
        P.op('act', 'copy', dict(out=gT[0:16, 0:1024], in_=pb[0:16, 0:1024]), [('ps', 1)], ['gT'])
        pb2 = PSB(2)
        for t in range(8, NT):
            P.op('pe', 'transpose', dict(out=pb2[0:16, (t - 8) * 128:(t - 7) * 128], in_=gates[:, t, :], identity=ident), ['gates', 'ident'], [('ps', 2)])
        P.op('act', 'copy', dict(out=gT[0:16, 1024:2048], in_=pb2[0:16, 0:1024]), [('ps', 2)], ['gT'])

        for eb in range(16 // EB):
            for ei in range(EB):
                ex_ = eb * EB + ei
                ws = wgu[ex_ % 2]
                for c in range(8):
                    P.dma('pool', ws[:, c, :], w_gu[l, ex_, c * 128:(c + 1) * 128, :], [], [('wgu', ex_ % 2)], ('wgu', ex_ % 2))
                P.dma('pool', wdn[:, ei], w_dn[l, ex_].rearrange("(c p) n -> p c n", p=128), [], [('wdn', ei)], ('wdn', ei))
                for G in range(4):
                    gc = slice(G * 512, (G + 1) * 512)
                    bg = 4 + (G % 2)
                    P.op('pe', 'matmul', dict(out=PS(bg), lhsT=sel_sb[0:16, ex_, :], rhs=gT[0:16, gc], start=True, stop=True),
                         ['sel', 'gT'], [('ps', bg)])
                    for fc in range(2):
                        s2 = (G * 2 + fc) % 2
                        bh, bu = 0 + 2 * s2, 1 + 2 * s2
                        for (bank, c0) in ((bh, fc * 128), (bu, 256 + fc * 128)):
                            for c in range(8):
                                P.op('pe', 'matmul', dict(out=PS(bank), lhsT=ws[:, c, c0:c0 + 128], rhs=x1T[:, c, gc],
                                                                                                  start=(c == 0), stop=(c == 7)),
                                     [('wgu', ex_ % 2), ('x1T', 4 * G), ('x1T', 4 * G + 1), ('x1T', 4 * G + 2), ('x1T', 4 * G + 3)], [('ps', bank)])
                        P.op('act', 'activation', dict(out=sgl[s2], in_=PS(bh), func=AF.Silu), [('ps', bh)], [('sgl', s2)])
                        P.op('dve', 'tensor_tensor', dict(out=tml[s2], in0=PS(bu), in1=sgl[s2], op=ALU.mult), [('ps', bu), ('sgl', s2)], [('tml', s2)])
                        P.op('dve', 'tensor_tensor', dict(out=aT4[:, ei, fc, gc], in0=tml[s2], in1=PS(bg), op=ALU.mult),
                             [('tml', s2), ('ps', bg)], [('aT', ei, G)])
            for t in range(NT):
                tc_ = slice(t * 128, (t + 1) * 128)
                for hf in range(2):
                    bank = 6 + hf
                    n = 0
                    for ei in range(EB):
                        for fc in range(2):
                            P.op('pe', 'matmul', dict(out=PS(bank), lhsT=aT4[:, ei, fc, tc_], rhs=wdn[:, ei, fc, hf * 512:(hf + 1) * 512],
                                                                                                  start=(n == 0), stop=(n == 2 * EB - 1)),
                                 [('aT', ei, t // 4), ('wdn', ei)], [('ps', bank)])
                            n += 1
                    P.op('dve', 'tensor_tensor', dict(out=yacc[:, t, hf * 512:(hf + 1) * 512], in0=yacc[:, t, hf * 512:(hf + 1) * 512],
                                                                                in1=PS(bank), op=ALU.add), [('yacc', t), ('ps', bank)], [('yacc', t)])
        P.barrier()
        def d_fin(t):
            tc_ = slice(t * 128, (t + 1) * 128)
            k2 = t % 2
            k3 = t % NB3
            P.op('pool', 'tensor_tensor', dict(out=zt[k3], in0=zn[k3], in1=ln_sb[:, 3, :], op=ALU.add), [('zn', k3), 'ln_sb'], [('zt', k3)])
            P.dma('sp', x_dst[tc_, :], zt[k3], [('zt', k3)], [], 'xout')
            if not last:
                P.op('act', 'copy', dict(out=zb[k2], in_=zt[k3]), [('zt', k3)], [('zb', k2)])
                transpose_to(zb[k2], ('zb', k2), 8, bufA[:, :, tc_], ('xT', t), 4 + k2)

        skew([lambda t: ln_stats(t, yacc[:, t, :], ('yacc', t)),
              lambda t: ln_norm(t, yacc[:, t, :], ('yacc', t), 2),
              d_fin], NT)
        P.barrier()

    except _Stop:
        P.barrier()
    P.emit()
    es.close()
    return nc


def _tables(j):
    bf = ml_dtypes.bfloat16
    tb = {}
    i_ = np.arange(NT)[:, None]
    t_ = np.arange(128)[None, :]
    pos = ((4 * i_ + j) * 128 + t_).astype(np.float64)
    inv64 = 10000.0 ** (-np.arange(0, 64, 2, dtype=np.float64) / 64)
    ang = pos[:, :, None] * inv64[None, None, :]
    tb["t_rcos"] = np.cos(ang).transpose(1, 0, 2).reshape(128, NT * 32).astype(np.float32)
    tb["t_rsin"] = np.sin(ang).transpose(1, 0, 2).reshape(128, NT * 32).astype(np.float32)
    inv32 = 10000.0 ** (-np.arange(0, 32, 2, dtype=np.float64) / 32)
    a2 = pos.reshape(-1)[None, :] * inv32[:, None]
    sc = 96.0 ** -0.5
    cq = np.ones((96, TOK)) * sc
    sq = np.zeros((96, TOK))
    cq[64:80] = np.cos(a2) * sc; cq[80:96] = np.cos(a2) * sc
    sq[64:80] = np.sin(a2) * sc; sq[80:96] = np.sin(a2) * sc
    tb["t_cosq"] = cq.astype(np.float32); tb["t_sinq"] = sq.astype(np.float32)
    tb["t_cosk"] = np.concatenate([np.cos(a2), np.cos(a2)], 0).astype(np.float32)
    tb["t_sink"] = np.concatenate([np.sin(a2), np.sin(a2)], 0).astype(np.float32)
    gam = 1.0 - 2.0 ** (-5.0 - np.arange(4, dtype=np.float64))
    jj = np.arange(128)[:, None]; ii = np.arange(128)[None, :]
    dec = np.zeros((128, 4, 128))
    for h in range(4):
        dec[:, h, :] = np.where(ii >= jj, gam[h] ** np.maximum(ii - jj, 0), 0.0) / 8.0
    tb["t_decay"] = dec.reshape(128, 512).astype(np.float32)
    p = np.arange(128)
    xit = np.zeros((128, 2, 128))
    for c in range(2):
        hh = 2 * c + p // 64
        xit[:, c, :] = gam[hh][:, None] ** (np.arange(128)[None, :] + 1.0)
    tb["t_xit"] = xit.reshape(128, 256).astype(np.float32)
    zeta = np.zeros((128, 4, 64))
    for h in range(4):
        zeta[:, h, :] = (gam[h] ** (127.0 - np.arange(128)))[:, None] / 8.0
    tb["t_zeta"] = zeta.reshape(128, 256).astype(np.float32)
    bd = np.zeros((128, 2, 128))
    for c in range(2):
        bd[:, c, :] = (p[:, None] // 64 == np.arange(128)[None, :] // 64)
    tb["t_bd"] = bd.reshape(128, 256).astype(np.float32)
    ct = np.zeros((128, 2, 5)); cs = np.zeros((128, 2, 5))
    for c in range(2):
        Dd = gam[2 * c + p // 64] ** 128.0
        ct[:, c, 0] = Dd ** 4
        for r in range(4):
            ct[:, c, 1 + r] = Dd ** (3 - r)
        cs[:, c, 0] = Dd ** j
        for r in range(4):
            cs[:, c, 1 + r] = Dd ** (j - 1 - r) if r < j else 0.0
    eye = np.eye(128)
    tb["t_diagt"] = (ct[:, :, :, None] * eye[:, None, None, :]).reshape(128, 1280).astype(np.float32)
    tb["t_diags"] = (cs[:, :, :, None] * eye[:, None, None, :]).reshape(128, 1280).astype(np.float32)
    kl = np.arange(128)[:, None]; ql = np.arange(128)[None, :]
    caus = (kl <= ql).astype(np.float32); upper = (kl > ql).astype(np.float32)
    sm = np.zeros((128, 5, 3, 128), np.float32)
    for c in range(4):
        if c == j:
            sm[:, c] = caus[:, None, :]
        elif c == j - 1:
            sm[:, c] = upper[:, None, :]
    if j == 0:
        sm[:, 4] = upper[:, None, :]
    tb["t_swam"] = sm.reshape(128, 5 * 384).astype(bf)
    mm = np.zeros((128, 4, 128), np.float32)
    for r in range(4):
        if r < j:
            mm[:, r] = 1.0
        elif r == j:
            mm[:, r] = caus
    tb["t_mlam"] = mm.reshape(128, 512).astype(bf)
    tb["t_ident"] = np.eye(128, dtype=np.float32).astype(bf)
    sel = np.zeros((16, 16, 128), np.float32)
    for e in range(16):
        sel[e, e, :] = 1.0
    tb["t_sel"] = sel.reshape(16, 16 * 128).astype(bf)
    return tb


_TAB = {}
_NC = {}


def _shared(inp, ls):
    f = np.float32
    d = {}
    d["w_in"] = np.ascontiguousarray(inp["w_in"][ls], f)
    d["w_out"] = np.ascontiguousarray(inp["w_out"][ls], f)
    d["w_uq"] = np.ascontiguousarray(inp["mla_w_uq"][ls], f)
    d["w_ukv"] = np.ascontiguousarray(inp["mla_w_ukv"][ls], f)
    d["w_gu"] = np.ascontiguousarray(inp["exp_w_gate_up"][ls], f)
    d["w_dn"] = np.ascontiguousarray(inp["exp_w_down"][ls], f)
    d["w_rt"] = np.ascontiguousarray(inp["router_w"], f)
    n = len(ls)
    lnp = np.stack([np.stack([inp["ln1_g"][l], inp["ln1_b"][l], inp["ln2_g"][l], inp["ln2_b"][l]], 0) for l in ls], 0)
    d["lnp"] = np.ascontiguousarray(np.broadcast_to(lnp[:, :, None, :], (n, 4, 128, D)), f)
    d["gnw"] = np.ascontiguousarray(np.broadcast_to(np.asarray(inp["ret_gn_w"])[ls][:, None, :], (n, 128, 256)), f)
    d["qnw"] = np.ascontiguousarray(np.asarray(inp["mla_q_norm_w"])[ls].reshape(n, 3, 128).transpose(0, 2, 1), f)
    d["kvnw"] = np.ascontiguousarray(np.asarray(inp["mla_kv_norm_w"])[ls].reshape(n, 2, 128).transpose(0, 2, 1), f)
    d["sinks"] = np.ascontiguousarray(np.asarray(inp["swa_sinks"])[ls].reshape(n, 1, 6), f)
    rb = np.tile(np.asarray(inp["router_bias"], f)[None, :], (NT, 1)).reshape(1, 256)
    d["rbias"] = np.ascontiguousarray(np.broadcast_to(rb, (128, 256)), f)
    return d


def _run(xs, inp, ls, final, stop=None):
    key = (len(ls), final, stop)
    if key not in _NC:
        _NC[key] = build(len(ls), final_to_out=True, stop=stop)
    nc = _NC[key]
    sh = _shared(inp, ls)
    maps = []
    for c in range(8):
        j = c % 4
        if j not in _TAB:
            _TAB[j] = _tables(j)
        m = dict(sh)
        m.update(_TAB[j])
        m["x"] = xs[c]
        maps.append(m)
    res = run_bass_kernel_spmd(nc, maps, core_ids=list(range(8)))
    return [np.asarray(r["y"], np.float32) for r in res.results]


FUSED = True


def kernel(**inputs):
    inp = {k: np.asarray(v) for k, v in inputs.items()}
    x = np.asarray(inp["x"], np.float32)
    xs = []
    for c in range(8):
        b, j = c // 4, c % 4
        xt = x[b].reshape(64, 128, D)[j::4]
        xs.append(np.ascontiguousarray(xt.reshape(TOK, D)))
    if FUSED:
        ys = _run(xs, inp, [0, 1], True)
    else:
        ys = _run(xs, inp, [0], True)
        ys = _run(ys, inp, [1], True)
    out = np.zeros((2, 64, 128, D), np.float32)
    for c in range(8):
        b, j = c // 4, c % 4
        out[b, j::4] = ys[c].reshape(NT, 128, D)
    return out.reshape(2, 8192, D)
```
